# Optimizing a Trainium2 kernel written in Bass

```python
import jax, jax.numpy as jnp
from jax import lax
import numpy as np

D_MODEL = 1024
BATCH = 4
SEQ = 4096
DEPTH = 2

N_GROUPS = 8
GROUP_DIM = D_MODEL // 16
MIX_W = N_GROUPS * GROUP_DIM
CONF_WIDTH = 31
LRU_CONV_WIDTH = 4
LRU_C = 8.0
SHORT_CONV_WIDTH = 3
MOBA_BLOCK = 256
MOBA_TOPK = 3
MOBA_QCHUNK = 32
ROPE_THETA = 10000.0
FFN_DIM = 2816
N_EXPERTS = 8
TOP_K = 2
EXPERT_DIM = 3584
EXPERT_ROWS = 256
EPS = 1e-6
N_EVEN = (DEPTH + 1) // 2
N_ODD = DEPTH // 2

kernel_name = "hybrid_conformer_rglru_shortconv_moba_moe"

F32 = jnp.float32


def rms_norm(x, g):
    x32 = x.astype(F32)
    y = x32 * lax.rsqrt(jnp.mean(x32 * x32, axis=-1, keepdims=True) + EPS)
    return (y * g.astype(F32)).astype(x.dtype)


def layer_norm(x, g, b):
    x32 = x.astype(F32)
    mu = jnp.mean(x32, axis=-1, keepdims=True)
    var = jnp.mean(jnp.square(x32 - mu), axis=-1, keepdims=True)
    y = (x32 - mu) * lax.rsqrt(var + EPS) * g.astype(F32) + b.astype(F32)
    return y.astype(x.dtype)


def causal_depthwise_conv(x, w, b=None):
    width, ch = w.shape
    xp = jnp.pad(x, ((0, 0), (width - 1, 0), (0, 0)))
    y = lax.conv_general_dilated(xp, w[:, None, :].astype(x.dtype), window_strides=(1,),
                                 padding='VALID', dimension_numbers=('NWC', 'WIO', 'NWC'),
                                 feature_group_count=ch)
    return y if b is None else y + b


def rope(x, pos):
    half = x.shape[-1] // 2
    inv = ROPE_THETA ** (-jnp.arange(half, dtype=F32) / half)
    ang = pos.astype(F32)[:, None] * inv[None, :]
    cos = jnp.cos(ang)[None, :, None, :]
    sin = jnp.sin(ang)[None, :, None, :]
    x32 = x.astype(F32)
    x1, x2 = x32[..., :half], x32[..., half:]
    return jnp.concatenate([x1 * cos - x2 * sin, x2 * cos + x1 * sin], axis=-1).astype(x.dtype)


def rg_lru(x, wa, ba, wx, bx, lam):
    bsz, s, ch = x.shape
    xg = x.reshape(bsz, s, N_GROUPS, GROUP_DIM)
    r = jax.nn.sigmoid(jnp.einsum('bshi,hij->bshj', xg, wa).reshape(bsz, s, ch) + ba)
    i = jax.nn.sigmoid(jnp.einsum('bshi,hij->bshj', xg, wx).reshape(bsz, s, ch) + bx)
    log_a = LRU_C * r.astype(F32) * jax.nn.log_sigmoid(lam.astype(F32))
    a = jnp.exp(log_a)
    mult = jnp.sqrt(-jnp.expm1(2.0 * log_a))
    bterm = mult * (i * x).astype(F32)

    def combine(left, right):
        a_l, b_l = left
        a_r, b_r = right
        return a_l * a_r, a_r * b_l + b_r

    _, h = lax.associative_scan(combine, (a, bterm), axis=1)
    return h.astype(x.dtype)


def mixer_conformer_rglru(h, w_in, conv_a_w, conv_a_b, ln_a_g, ln_a_b, conv_b_w, conv_b_b,
                          lru_wa, lru_ba, lru_wx, lru_bx, lru_lambda, w_out):
    u = h @ w_in
    a_val, a_gate, b_x, b_gate = jnp.split(u, 4, axis=-1)
    ya = a_val * jax.nn.sigmoid(a_gate)
    ya = causal_depthwise_conv(ya, conv_a_w, conv_a_b)
    ya = jax.nn.silu(layer_norm(ya, ln_a_g, ln_a_b))
    xb = causal_depthwise_conv(b_x, conv_b_w, conv_b_b)
    yb = rg_lru(xb, lru_wa, lru_ba, lru_wx, lru_bx, lru_lambda) * jax.nn.gelu(b_gate)
    return jnp.concatenate([ya, yb], axis=-1) @ w_out


def moba_attention(q, k, v):
    bsz, s, nh, dh = q.shape
    s_pad = -(-s // MOBA_BLOCK) * MOBA_BLOCK
    padw = ((0, 0), (0, s_pad - s), (0, 0), (0, 0))
    q, k, v = jnp.pad(q, padw), jnp.pad(k, padw), jnp.pad(v, padw)
    nb = s_pad // MOBA_BLOCK
    nc = s_pad // MOBA_QCHUNK
    n_topk = min(MOBA_TOPK, nb)
    scale = dh ** -0.5
    kb = k.transpose(0, 2, 1, 3).reshape(bsz, nh, nb, MOBA_BLOCK, dh)
    vb = v.transpose(0, 2, 1, 3).reshape(bsz, nh, nb, MOBA_BLOCK, dh)
    k_mean = jnp.mean(kb.astype(F32), axis=3)
    q_chunks = q.transpose(0, 2, 1, 3).reshape(bsz, nh, nc, MOBA_QCHUNK, dh).transpose(2, 0, 1, 3, 4)
    b_idx = jnp.arange(bsz)[:, None, None, None]
    h_idx = jnp.arange(nh)[None, :, None, None]
    blk_ids = jnp.arange(nb)
    slot = jnp.arange(n_topk)
    within = jnp.arange(MOBA_BLOCK)
    n_sel = n_topk * MOBA_BLOCK

    def one_chunk(args):
        qc, ci = args
        q_start = ci * MOBA_QCHUNK
        own = q_start // MOBA_BLOCK
        q_pos = q_start + jnp.arange(MOBA_QCHUNK)
        gate = jnp.einsum('bhqd,bhnd->bhqn', qc.astype(F32), k_mean)
        gate = jnp.where(blk_ids < own, gate, -jnp.inf)
        _, sel = lax.top_k(gate, n_topk)
        sel_valid = slot < own
        k_sel = kb[b_idx, h_idx, sel]
        v_sel = vb[b_idx, h_idx, sel]
        s_sel = jnp.einsum('bhqd,bhqkjd->bhqkj', qc, k_sel).astype(F32) * scale
        s_sel = jnp.where(sel_valid[:, None], s_sel, -jnp.inf)
        k_own = lax.dynamic_index_in_dim(kb, own, axis=2, keepdims=False)
        v_own = lax.dynamic_index_in_dim(vb, own, axis=2, keepdims=False)
        s_own = jnp.einsum('bhqd,bhjd->bhqj', qc, k_own).astype(F32) * scale
        causal = (own * MOBA_BLOCK + within)[None, :] <= q_pos[:, None]
        s_own = jnp.where(causal, s_own, -jnp.inf)
        scores = jnp.concatenate([s_sel.reshape(bsz, nh, MOBA_QCHUNK, n_sel), s_own], axis=-1)
        p = jax.nn.softmax(scores, axis=-1)
        p_sel = p[..., :n_sel].reshape(bsz, nh, MOBA_QCHUNK, n_topk, MOBA_BLOCK).astype(v.dtype)
        p_own = p[..., n_sel:].astype(v.dtype)
        return (jnp.einsum('bhqkj,bhqkjd->bhqd', p_sel, v_sel)
                + jnp.einsum('bhqj,bhjd->bhqd', p_own, v_own))

    out = lax.map(one_chunk, (q_chunks, jnp.arange(nc)))
    out = out.transpose(1, 0, 3, 2, 4).reshape(bsz, s_pad, nh, dh)
    return out[:, :s]


def mixer_shortconv_moba(h, w_in, conv_c_w, q_norm, k_norm, w_out, pos):
    bsz, s, _ = h.shape
    u = h @ w_in
    c_h, c_b, c_c, q, k, v = jnp.split(u, 6, axis=-1)
    yc = c_b * causal_depthwise_conv(c_c * c_h, conv_c_w)
    q = rope(rms_norm(q.reshape(bsz, s, N_GROUPS, GROUP_DIM), q_norm), pos)
    k = rope(rms_norm(k.reshape(bsz, s, N_GROUPS, GROUP_DIM), k_norm), pos)
    v = v.reshape(bsz, s, N_GROUPS, GROUP_DIM)
    yd = moba_attention(q, k, v).reshape(bsz, s, MIX_W)
    return jnp.concatenate([yc, yd], axis=-1) @ w_out


def swiglu(h, wg, wu, wd):
    return (jax.nn.silu(h @ wg) * (h @ wu)) @ wd


def moe_swiglu(h, router_w, router_b, wg, wu, wd):
    bsz, s, d = h.shape
    n_tok = bsz * s
    xf = h.reshape(n_tok, d)
    logits = xf.astype(F32) @ router_w.astype(F32) + router_b.astype(F32)
    top_logit, top_e = lax.top_k(logits, TOP_K)
    top_w = jax.nn.softmax(top_logit, axis=-1)
    n_assign = n_tok * TOP_K
    flat_e = top_e.reshape(-1)
    flat_tok = jnp.repeat(jnp.arange(n_tok, dtype=jnp.int32), TOP_K)
    flat_w = top_w.reshape(-1)
    order = jnp.argsort(flat_e)
    sorted_e = flat_e[order]
    counts = jnp.bincount(flat_e, length=N_EXPERTS)
    padded = (counts + EXPERT_ROWS - 1) // EXPERT_ROWS * EXPERT_ROWS
    start = jnp.cumsum(counts) - counts
    pend = jnp.cumsum(padded)
    pstart = pend - padded
    dest = pstart[sorted_e] + (jnp.arange(n_assign) - start[sorted_e])
    n_rows = -(-(n_assign + N_EXPERTS * (EXPERT_ROWS - 1)) // EXPERT_ROWS) * EXPERT_ROWS
    n_blocks = n_rows // EXPERT_ROWS
    row_tok = jnp.zeros((n_rows,), jnp.int32).at[dest].set(flat_tok[order])
    row_w = jnp.zeros((n_rows,), F32).at[dest].set(flat_w[order])
    blk_e = jnp.minimum(jnp.searchsorted(pend, jnp.arange(n_blocks) * EXPERT_ROWS, side='right'),
                        N_EXPERTS - 1)
    xs = xf[row_tok].reshape(n_blocks, EXPERT_ROWS, d)

    def expert_block(args):
        xb, e = args
        return (jax.nn.silu(xb @ wg[e]) * (xb @ wu[e])) @ wd[e]

    ys = lax.map(expert_block, (xs, blk_e)).reshape(n_rows, d)
    out = jnp.zeros((n_tok, d), F32).at[row_tok].add(ys.astype(F32) * row_w[:, None])
    return out.astype(h.dtype).reshape(bsz, s, d)


def setup_inputs(seed: int = 0) -> dict:
    key = jax.random.key(seed)
    ks = iter(jax.random.split(key, 48))
    D = D_MODEL

    def nrm(shape, scale):
        return jax.random.normal(next(ks), shape, jnp.float32) * scale

    a0 = jax.random.uniform(next(ks), (N_EVEN, MIX_W), jnp.float32, 0.9, 0.999)
    return {
        "x": nrm((BATCH, SEQ, D), 1.0),
        "c": nrm((BATCH, D), 1.0),
        "e_ada_w": nrm((N_EVEN, D, 6 * D), 0.5 * D ** -0.5),
        "e_ada_b": nrm((N_EVEN, 6 * D), 0.02),
        "e_norm_mix": 1.0 + nrm((N_EVEN, D), 0.05),
        "e_norm_ffn": 1.0 + nrm((N_EVEN, D), 0.05),
        "e_w_in": nrm((N_EVEN, D, 4 * MIX_W), D ** -0.5),
        "e_conv_a_w": nrm((N_EVEN, CONF_WIDTH, MIX_W), CONF_WIDTH ** -0.5),
        "e_conv_a_b": nrm((N_EVEN, MIX_W), 0.02),
        "e_ln_a_g": 1.0 + nrm((N_EVEN, MIX_W), 0.05),
        "e_ln_a_b": nrm((N_EVEN, MIX_W), 0.02),
        "e_conv_b_w": nrm((N_EVEN, LRU_CONV_WIDTH, MIX_W), LRU_CONV_WIDTH ** -0.5),
        "e_conv_b_b": nrm((N_EVEN, MIX_W), 0.02),
        "e_lru_wa": nrm((N_EVEN, N_GROUPS, GROUP_DIM, GROUP_DIM), GROUP_DIM ** -0.5),
        "e_lru_ba": nrm((N_EVEN, MIX_W), 0.02),
        "e_lru_wx": nrm((N_EVEN, N_GROUPS, GROUP_DIM, GROUP_DIM), GROUP_DIM ** -0.5),
        "e_lru_bx": nrm((N_EVEN, MIX_W), 0.02),
        "e_lru_lambda": jnp.log(a0) - jnp.log1p(-a0),
        "e_w_out": nrm((N_EVEN, 2 * MIX_W, D), (2 * MIX_W) ** -0.5),
        "e_ffn_wg": nrm((N_EVEN, D, FFN_DIM), D ** -0.5),
        "e_ffn_wu": nrm((N_EVEN, D, FFN_DIM), D ** -0.5),
        "e_ffn_wd": nrm((N_EVEN, FFN_DIM, D), FFN_DIM ** -0.5),
        "o_ada_w": nrm((N_ODD, D, 6 * D), 0.5 * D ** -0.5),
        "o_ada_b": nrm((N_ODD, 6 * D), 0.02),
        "o_norm_mix": 1.0 + nrm((N_ODD, D), 0.05),
        "o_norm_ffn": 1.0 + nrm((N_ODD, D), 0.05),
        "o_w_in": nrm((N_ODD, D, 6 * MIX_W), D ** -0.5),
        "o_conv_c_w": nrm((N_ODD, SHORT_CONV_WIDTH, MIX_W), SHORT_CONV_WIDTH ** -0.5),
        "o_q_norm": 1.0 + nrm((N_ODD, GROUP_DIM), 0.05),
        "o_k_norm": 1.0 + nrm((N_ODD, GROUP_DIM), 0.05),
        "o_w_out": nrm((N_ODD, 2 * MIX_W, D), (2 * MIX_W) ** -0.5),
        "o_router_w": nrm((N_ODD, D, N_EXPERTS), D ** -0.5),
        "o_router_b": nrm((N_ODD, N_EXPERTS), 0.01),
        "o_moe_wg": nrm((N_ODD, N_EXPERTS, D, EXPERT_DIM), D ** -0.5),
        "o_moe_wu": nrm((N_ODD, N_EXPERTS, D, EXPERT_DIM), D ** -0.5),
        "o_moe_wd": nrm((N_ODD, N_EXPERTS, EXPERT_DIM, D), EXPERT_DIM ** -0.5),
    }


def reference(x, c, e_ada_w, e_ada_b, e_norm_mix, e_norm_ffn, e_w_in, e_conv_a_w, e_conv_a_b,
              e_ln_a_g, e_ln_a_b, e_conv_b_w, e_conv_b_b, e_lru_wa, e_lru_ba, e_lru_wx, e_lru_bx,
              e_lru_lambda, e_w_out, e_ffn_wg, e_ffn_wu, e_ffn_wd,
              o_ada_w, o_ada_b, o_norm_mix, o_norm_ffn, o_w_in, o_conv_c_w, o_q_norm, o_k_norm,
              o_w_out, o_router_w, o_router_b, o_moe_wg, o_moe_wu, o_moe_wd):
    pos = jnp.arange(x.shape[1])
    c_act = jax.nn.silu(c)
    for layer in range(DEPTH):
        j = layer // 2
        if layer % 2 == 0:
            mod = (c_act @ e_ada_w[j] + e_ada_b[j])[:, None, :]
            sh1, sc1, g1, sh2, sc2, g2 = jnp.split(mod, 6, axis=-1)
            h = rms_norm(x, e_norm_mix[j]) * (1.0 + sc1) + sh1
            x = x + g1 * mixer_conformer_rglru(
                h, e_w_in[j], e_conv_a_w[j], e_conv_a_b[j], e_ln_a_g[j], e_ln_a_b[j],
                e_conv_b_w[j], e_conv_b_b[j], e_lru_wa[j], e_lru_ba[j], e_lru_wx[j], e_lru_bx[j],
                e_lru_lambda[j], e_w_out[j])
            h = rms_norm(x, e_norm_ffn[j]) * (1.0 + sc2) + sh2
            x = x + g2 * swiglu(h, e_ffn_wg[j], e_ffn_wu[j], e_ffn_wd[j])
        else:
            mod = (c_act @ o_ada_w[j] + o_ada_b[j])[:, None, :]
            sh1, sc1, g1, sh2, sc2, g2 = jnp.split(mod, 6, axis=-1)
            h = rms_norm(x, o_norm_mix[j]) * (1.0 + sc1) + sh1
            x = x + g1 * mixer_shortconv_moba(h, o_w_in[j], o_conv_c_w[j], o_q_norm[j], o_k_norm[j],
                                              o_w_out[j], pos)
            h = rms_norm(x, o_norm_ffn[j]) * (1.0 + sc2) + sh2
            x = x + g2 * moe_swiglu(h, o_router_w[j], o_router_b[j], o_moe_wg[j], o_moe_wu[j],
                                    o_moe_wd[j])
    return x
```

```python
import numpy as np
from contextlib import ExitStack
import concourse.bass as bass
import concourse.mybir as mybir

F32 = mybir.dt.float32
BF16 = mybir.dt.bfloat16
AF = mybir.ActivationFunctionType
ALU = mybir.AluOpType
AX = mybir.AxisListType

ENGS = ["pe", "act", "dve", "pool", "sp"]
EPOCH = 6000
NDSEM = 14
ARENA_BYTES = 207 * 1024


class Buf:
    __slots__ = ("w", "r")

    def __init__(self):
        self.w = None
        self.r = set()


class V:
    __slots__ = ("ap", "bufs")

    def __init__(self, ap, bufs):
        self.ap = ap
        self.bufs = bufs


class Tens:
    def __init__(self, full_ap, P, C, N, tile):
        self.P, self.C, self.N = P, C, N
        self.tile = tile
        nt = (N + tile - 1) // tile
        self.bufs = [[Buf() for _ in range(nt)] for _ in range(C)]
        self.full = full_ap

    def allbufs(self):
        return [b for bl in self.bufs for b in bl]

    def v(self, c, lo=0, hi=None, p0=0, p1=None):
        if hi is None:
            hi = self.N
        if p1 is None:
            p1 = self.P
        ap = self.full[p0:p1, c * self.N + lo: c * self.N + hi]
        bl = self.bufs[c][lo // self.tile: (hi - 1) // self.tile + 1]
        return V(ap, list(bl))

    def vc(self, c0, c1, lo=0, hi=None, p0=0, p1=None):
        if hi is None:
            hi = self.N
        if p1 is None:
            p1 = self.P
        ap = self.full[p0:p1, c0 * self.N: c1 * self.N].rearrange("p (c n) -> p c n", c=c1 - c0)[:, :, lo:hi]
        bl = []
        for c in range(c0, c1):
            bl += self.bufs[c][lo // self.tile: (hi - 1) // self.tile + 1]
        return V(ap, bl)

    def all(self):
        return V(self.full, self.allbufs())


def _compress(deps, ops):
    best = {}
    out = set()
    for (e, i) in deps:
        if ops[e][i]["dma"]:
            out.add((e, i))
        else:
            if e not in best or best[e] < i:
                best[e] = i
    for e, i in best.items():
        out.add((e, i))
    return out


class Prog:
    def __init__(self, nc, es):
        self.nc = nc
        self.es = es
        self.ops = {e: [] for e in ENGS}
        self.ndma = {"sp": 0, "pool": 0}
        self.arena = es.enter_context(nc.sbuf_tensor("arena", [128, ARENA_BYTES // 2], BF16))
        self.arena_ap = self.arena[:]
        self.top = 0
        self.live = []
        self.retired = []
        self.banks = []
        for i in range(8):
            h = es.enter_context(nc.psum_tensor(f"bank{i}", [128, 512], F32))
            self.banks.append(Tens(h[:], 128, 1, 512, 512))
        self.free_banks = list(range(8))

    def acquire(self):
        assert self.free_banks, "out of PSUM banks"
        i = self.free_banks.pop(0)
        return i

    def release(self, i):
        self.free_banks.append(i)

    def alloc(self, C, N, dtype, tile=None, P=128, at=None):
        esz = 4 if dtype == F32 else 2
        nbytes = C * N * esz
        nbytes_al = (nbytes + 31) // 32 * 32
        if at is None:
            lo = self.top
            self.top += nbytes_al
        else:
            lo = at
        hi = lo + nbytes_al
        assert hi <= ARENA_BYTES, f"arena overflow {hi}"
        for (l2, h2, _) in self.live:
            assert hi <= l2 or lo >= h2, f"overlap with live tensor [{l2},{h2}) vs [{lo},{hi})"
        ap = self.arena_ap[0:P, lo // 2: (lo + nbytes) // 2]
        if dtype == F32:
            ap = ap.bitcast(F32)
        t = Tens(ap, P, C, N, tile or N)
        seeds = set()
        keep = []
        for (l2, h2, t2) in self.retired:
            if hi <= l2 or lo >= h2:
                keep.append((l2, h2, t2))
                continue
            for b in t2.allbufs():
                if b.w is not None:
                    seeds.add(b.w)
                seeds |= b.r
            if not (lo <= l2 and h2 <= hi):
                keep.append((l2, h2, t2))
        self.retired = keep
        if seeds:
            seeds = _compress(seeds, self.ops)
            for b in t.allbufs():
                b.r = set(seeds)
        self.live.append((lo, hi, t))
        return t

    def mark(self):
        return (self.top, len(self.live))

    def reset(self, mark):
        top, n = mark
        for ent in self.live[n:]:
            self.retired.append(ent)
        self.live = self.live[:n]
        self.top = top

    def free(self, t):
        for k, ent in enumerate(self.live):
            if ent[2] is t:
                self.retired.append(ent)
                del self.live[k]
                return
        raise KeyError

    def op(self, eng, fn, reads=(), writes=(), dma=False):
        idx = len(self.ops[eng])
        deps = set()
        for v in reads:
            for b in v.bufs:
                if b.w is not None:
                    deps.add(b.w)
        for v in writes:
            for b in v.bufs:
                if b.w is not None:
                    deps.add(b.w)
                deps |= b.r
        me = (eng, idx)
        deps.discard(me)
        if eng == "pe":
            deps = {d for d in deps if d[0] != "pe"}
        deps = _compress(deps, self.ops)
        o = {"fn": fn, "deps": deps, "sig": False, "dma": dma}
        if dma:
            q = self.ndma[eng]
            self.ndma[eng] += 1
            o["dq"] = q
        self.ops[eng].append(o)
        for v in reads:
            for b in v.bufs:
                if not dma:
                    b.r = {x for x in b.r if x[0] != eng or self.ops[eng][x[1]]["dma"]}
                b.r.add(me)
        for v in writes:
            for b in v.bufs:
                b.w = me
                b.r = set()
        return me

    def emit(self):
        nc = self.nc
        ops = self.ops
        for e in ENGS:
            for o in ops[e]:
                for (de, di) in o["deps"]:
                    ops[de][di]["sig"] = True
        nsig = {}
        for e in ENGS:
            r = 0
            for i, o in enumerate(ops[e]):
                if o["sig"] and not o["dma"]:
                    r += 1
                    o["rank"] = r
            nsig[e] = r
        es = self.es
        esem = {}
        for e in ENGS:
            n_ep = (nsig[e] + EPOCH - 1) // EPOCH
            esem[e] = [es.enter_context(nc.semaphore(f"s_{e}_{k}")) for k in range(max(n_ep, 1))]
        dsem = {q: [es.enter_context(nc.semaphore(f"d_{q}_{k}")) for k in range(NDSEM)] for q in ("sp", "pool")}
        block = es.enter_context(nc.Block())
        self.stats = {e: len(ops[e]) for e in ENGS}
        self.stats["nsig"] = nsig

        def run(eng, e):
            waited_rank = {x: 0 for x in ENGS}
            waited_dma = {}
            for o in ops[eng]:
                for (de, di) in sorted(o["deps"]):
                    d = ops[de][di]
                    if d["dma"]:
                        q = d["dq"]
                        k = q % NDSEM
                        val = 16 * (q // NDSEM + 1)
                        if waited_dma.get((de, k), 0) < val:
                            e.wait_ge(dsem[de][k], val)
                            waited_dma[(de, k)] = val
                    else:
                        r = d["rank"]
                        if waited_rank[de] < r:
                            ep = (r - 1) // EPOCH
                            e.wait_ge(esem[de][ep], r - ep * EPOCH)
                            waited_rank[de] = r
                if o["dma"]:
                    q = o["dq"]
                    k = q % NDSEM
                    prev = 16 * (q // NDSEM)
                    if prev > 0 and waited_dma.get((eng, k), 0) < prev:
                        e.wait_ge(dsem[eng][k], prev)
                        waited_dma[(eng, k)] = prev
                    ins = o["fn"](e)
                    ins.then_inc(dsem[eng][k], 16)
                elif o["fn"] is not None:
                    ins = o["fn"](e)
                    if o["sig"]:
                        r = o["rank"]
                        ep = (r - 1) // EPOCH
                        ins.then_inc(esem[eng][ep], 1)

        @block.tensor
        def _(e):
            run("pe", e)

        @block.scalar
        def _(e):
            run("act", e)

        @block.vector
        def _(e):
            run("dve", e)

        @block.gpsimd
        def _(e):
            run("pool", e)

        @block.sync
        def _(e):
            run("sp", e)

    def mm(self, out, lhsT, rhs, start=True, stop=True):
        return self.op("pe", lambda e: e.matmul(out.ap, lhsT.ap, rhs.ap, start=start, stop=stop),
                       reads=[lhsT, rhs], writes=[out])

    def transpose(self, out, in_, ident):
        return self.op("pe", lambda e: e.transpose(out.ap, in_.ap, ident.ap), reads=[in_, ident], writes=[out])

    def act(self, out, in_, func, scale=1.0, bias=0.0):
        reads = [in_]
        sc = scale
        bi = bias
        if isinstance(scale, V):
            reads.append(scale)
            sc = scale.ap
        if isinstance(bias, V):
            reads.append(bias)
            bi = bias.ap
        return self.op("act", lambda e: e.activation(out.ap, in_.ap, func, bias=bi, scale=sc),
                       reads=reads, writes=[out])

    def tt(self, out, a, b, op, eng="dve"):
        return self.op(eng, lambda e: e.tensor_tensor(out.ap, a.ap, b.ap, op), reads=[a, b], writes=[out])

    def ts(self, out, a, s1, op0, s2=None, op1=None, eng="dve"):
        reads = [a]
        x1 = s1
        x2 = s2
        if isinstance(s1, V):
            reads.append(s1)
            x1 = s1.ap
        if isinstance(s2, V):
            reads.append(s2)
            x2 = s2.ap
        if op1 is None:
            return self.op(eng, lambda e: e.tensor_scalar(out.ap, a.ap, x1, None, op0), reads=reads, writes=[out])
        return self.op(eng, lambda e: e.tensor_scalar(out.ap, a.ap, x1, x2, op0, op1), reads=reads, writes=[out])

    def stt(self, out, a, s, b, op0, op1):
        reads = [a, b]
        x = s
        if isinstance(s, V):
            reads.append(s)
            x = s.ap
        return self.op("dve", lambda e: e.scalar_tensor_tensor(out.ap, a.ap, x, b.ap, op0, op1), reads=reads, writes=[out])

    def copy(self, out, a, eng="dve"):
        if eng == "act":
            return self.act(out, a, AF.Identity)
        return self.op(eng, lambda e: e.tensor_copy(out.ap, a.ap), reads=[a], writes=[out])

    def memset(self, out, val, eng="dve"):
        return self.op(eng, lambda e: e.memset(out.ap, val), reads=[], writes=[out])

    def reduce(self, out, a, op, eng="dve"):
        return self.op(eng, lambda e: e.tensor_reduce(out.ap, a.ap, AX.X, op), reads=[a], writes=[out])

    def recip(self, out, a):
        return self.op("dve", lambda e: e.reciprocal(out.ap, a.ap), reads=[a], writes=[out])

    def scan(self, out, d0, d1, init):
        reads = [d0, d1]
        x = init
        if isinstance(init, V):
            reads.append(init)
            x = init.ap
        return self.op("dve", lambda e: e.tensor_tensor_scan(out.ap, d0.ap, d1.ap, x, ALU.mult, ALU.add),
                       reads=reads, writes=[out])

    def dma(self, out, in_, q="sp"):
        reads = [in_] if isinstance(in_, V) else []
        writes = [out] if isinstance(out, V) else []
        oa = out.ap if isinstance(out, V) else out
        ia = in_.ap if isinstance(in_, V) else in_
        return self.op(q, lambda e: e.dma_start(out=oa, in_=ia), reads=reads, writes=writes, dma=True)

    def finish(self, out_dma_ops):
        deps = set(out_dma_ops)
        self.ops["sp"].append({"fn": None, "deps": deps, "sig": False, "dma": False})

T = 2048
TT = 512
NT = T // TT
BIG = 30000.0
EPS = 1e-6

PE_OFF = {}
_o = 0
for _n, _w in [("nm", 8), ("nf", 8), ("adab", 48), ("caw", 124), ("cab", 4), ("lng", 4), ("lnb", 4),
               ("cbw", 16), ("cbb", 4), ("lba", 4), ("lbx", 4), ("lam", 4)]:
    PE_OFF[_n] = _o
    _o += _w
NPE = _o
PO_OFF = {}
_o = 0
for _n, _w in [("nm", 8), ("nf", 8), ("adab", 48), ("ccw", 12), ("qn", 1), ("kn", 1), ("rb", 8)]:
    PO_OFF[_n] = _o
    _o += _w
NPO = _o


class RR:
    def __init__(self, tiles):
        self.t = tiles
        self.i = 0

    def get(self):
        x = self.t[self.i % len(self.t)]
        self.i += 1
        return x


def build_program(stage=99):
    nc = bass.Bass("TRN2", target_bir_lowering=False)

    def din(name, shape):
        return nc.dram_tensor(name, shape, F32, kind="ExternalInput").ap()

    xA = din("xA", [1024, T])
    xB = din("xB", [1024, T])
    cvec = din("cvec", [128, 8])
    flag_d = din("flag", [128, 1])
    pe_d = din("pe", [128, NPE])
    po_d = din("po", [128, NPO])
    c32_d = din("c32", [128, 258])
    cb_d = din("cb", [128, 6 * 128])
    cc_d = din("cc", [128, 896])
    ej_d = din("ej", [128, 2048])
    blkb_d = din("blkb", [128, 16])
    cos_d = din("cos", [128, 2 * T])
    sin_d = din("sin", [128, 2 * T])
    e_ada_w = din("e_ada_w", [1024, 6144])
    o_ada_w = din("o_ada_w", [1024, 6144])
    e_w_in = din("e_w_in", [1024, 2048])
    e_w_out = din("e_w_out", [1024, 1024])
    lru_w = din("lru_w", [128, 8 * 128])
    e_wg = din("e_ffn_wg", [1024, 2816])
    e_wu = din("e_ffn_wu", [1024, 2816])
    e_wd = din("e_ffn_wd", [2816, 1024])
    o_w_in = din("o_w_in", [1024, 3072])
    o_w_out = din("o_w_out", [1024, 1024])
    o_rw = din("o_router_w", [1024, 8])
    if stage >= 5:
        m_wg = din("o_moe_wg", [8 * 1024, 3584])
        m_wu = din("o_moe_wu", [8 * 1024, 3584])
        m_wd = din("o_moe_wd", [8 * 3584, 1024])
    out_d = nc.dram_tensor("out", [1024, T], F32, kind="ExternalOutput").ap()

    es = ExitStack()
    with es:
        P = Prog(nc, es)
        X = P.alloc(8, T, F32, tile=TT)
        CB = P.alloc(6, 128, BF16)
        CC = P.alloc(1, 896, BF16)
        EJ = P.alloc(16, 128, BF16)
        C32 = P.alloc(1, 258, F32)
        PEt = P.alloc(1, NPE, F32)
        POt = P.alloc(1, NPO, F32)
        FLG = P.alloc(1, 1, F32)
        BLKB = P.alloc(1, 16, F32)
        CACT = P.alloc(1, 8, BF16)
        MOD = [P.alloc(1, 48, F32), P.alloc(1, 48, F32)]
        DER = P.alloc(1, 64, F32)
        LST = P.alloc(4, 1, F32)
        YAH = P.alloc(4, 30, BF16)
        BXH = P.alloc(4, 3, BF16)
        ZH = P.alloc(4, 2, BF16)
        KMT = P.alloc(4, 16, F32)
        KMTB = P.alloc(4, 16, BF16)
        KMH = [P.alloc(4, 16, BF16), P.alloc(4, 16, BF16)]
        WTOK = P.alloc(1, 128, F32)
        LRUW = P.alloc(8, 128, BF16)
        RW = P.alloc(8, 8, F32)

        identb, ones1024, ones512, blk64, onesb, rperm = [CB.v(i) for i in range(6)]
        ident32, ones32 = C32.v(0, 0, 128), C32.v(0, 128, 256)
        hmask = [C32.v(0, 256, 257), C32.v(0, 257, 258)]

        def pe_col(name, j=0, n=1):
            o = PE_OFF[name] + j
            return PEt.v(0, o, o + n)

        def po_col(name, j=0, n=1):
            o = PO_OFF[name] + j
            return POt.v(0, o, o + n)

        def der(j, n=1):
            return DER.v(0, j, j + n)

        P.dma(CB.all(), cb_d, q="pool")
        P.dma(CC.all(), cc_d, q="pool")
        P.dma(EJ.all(), ej_d, q="pool")
        P.dma(LRUW.all(), lru_w, q="pool")
        P.dma(C32.all(), c32_d)
        P.dma(PEt.all(), pe_d)
        P.dma(POt.all(), po_d)
        P.dma(FLG.all(), flag_d)
        P.dma(BLKB.all(), blkb_d)
        P.dma(RW.vc(0, 8), o_rw.rearrange("(k p) e -> p k e", p=128))
        P.memset(KMTB.all(), 0.0)
        P.memset(KMH[0].all(), 0.0)
        P.memset(KMH[1].all(), 0.0)

        m0 = P.mark()
        CV = P.alloc(1, 8, F32)
        P.dma(CV.all(), cvec)
        P.act(CACT.all(), CV.all(), AF.Silu)
        def compute_mod(L):
            mkm = P.mark()
            WS = [P.alloc(8, 1024, BF16), P.alloc(8, 1024, BF16)]
            wi = 0
            adaw = [e_ada_w, o_ada_w][L]
            bk = P.acquire()
            bank = P.banks[bk]
            adaw_r = adaw.rearrange("(k p) n -> p k n", p=128)
            for v6 in range(6):
                w = WS[wi % 2]
                wi += 1
                P.dma(w.vc(0, 8), adaw_r[:, :, v6 * 1024:(v6 + 1) * 1024], q="pool")
                for mc in range(8):
                    col = v6 * 8 + mc
                    for k in range(8):
                        P.mm(bank.v(0, col, col + 1), w.v(k, mc * 128, (mc + 1) * 128), CACT.v(0, k, k + 1),
                             start=(k == 0), stop=(k == 7))
            par = PEt if L == 0 else POt
            off = (PE_OFF if L == 0 else PO_OFF)["adab"]
            P.tt(MOD[L].all(), bank.v(0, 0, 48), par.v(0, off, off + 48), ALU.add)
            P.release(bk)
            offm = (PE_OFF if L == 0 else PO_OFF)
            P.stt(der(L * 16, 8), MOD[L].v(0, 8, 16), 1.0, par.v(0, offm["nm"], offm["nm"] + 8), ALU.add, ALU.mult)
            P.stt(der(L * 16 + 8, 8), MOD[L].v(0, 32, 40), 1.0, par.v(0, offm["nf"], offm["nf"] + 8), ALU.add, ALU.mult)
            P.reset(mkm)

        compute_mod(0)
        lam = pe_col("lam", 0, 4)
        t_abs = der(40, 4)
        t_e = der(44, 4)
        t_mx = der(48, 4)
        P.ts(t_mx, lam, -1.0, ALU.mult)
        P.tt(t_abs, lam, t_mx, ALU.max)
        P.act(t_e, t_abs, AF.Exp, scale=-1.0)
        P.act(t_e, t_e, AF.Ln, bias=1.0)
        P.ts(t_mx, lam, -1.0, ALU.mult, 0.0, ALU.max)
        P.tt(t_e, t_e, t_mx, ALU.add)
        P.ts(der(32, 4), t_e, -8.0, ALU.mult)
        P.ts(der(36, 4), t_e, -16.0, ALU.mult)
        P.reset(m0)

        A1 = [lambda k, L=L: der(L * 16 + k) for L in range(2)]
        A2 = [lambda k, L=L: der(L * 16 + 8 + k) for L in range(2)]

        def modc(L, which, k):
            return MOD[L].v(0, which * 8 + k, which * 8 + k + 1)

        def rstd_from_bank(bank, outR):
            P.act(outR, bank, AF.Ln, bias=EPS)
            P.act(outR, outR, AF.Exp, scale=-0.5)

        RN = P.alloc(1, TT, F32)

        def norm_tile(lo, Acol, shcol, hout, s32, s16, h32cb=None):
            bk = P.acquire()
            bank = P.banks[bk].v(0)
            for k in range(8):
                sq = s16.get().v(0)
                P.act(sq, X.v(k, lo, lo + TT), AF.Square)
                P.mm(bank, ones1024, sq, start=(k == 0), stop=(k == 7))
            R = RN.v(0)
            rstd_from_bank(bank, R)
            P.release(bk)
            for k in range(8):
                t = s32.get().v(0)
                P.tt(t, X.v(k, lo, lo + TT), R, ALU.mult)
                P.act(hout(k), t, AF.Identity, scale=Acol(k), bias=shcol(k))
                if h32cb is not None:
                    h32cb(k, t)

        def stream_cols(w_d, c0, c1, dst):
            P.dma(dst.vc(0, 8, 0, c1 - c0), w_d.rearrange("(k p) n -> p k n", p=128)[:, :, c0:c1], q="pool")

        def proj_group(h, w, nchunk, N=TT):
            bks = []
            for c in range(nchunk):
                bk = P.acquire()
                for k in range(8):
                    P.mm(P.banks[bk].v(0, 0, N), w.v(k, c * 128, (c + 1) * 128), h(k), start=(k == 0), stop=(k == 7))
                bks.append(bk)
            return bks

        def out_proj(w_d, mix, gcol, lo, wbuf):
            wbuf_in = wbuf
            for half in range(2):
                wbuf = wbuf_in() if callable(wbuf_in) else wbuf_in
                stream_cols(w_d, half * 512, (half + 1) * 512, wbuf)
                for dc in range(4):
                    d = half * 4 + dc
                    bk = P.acquire()
                    for k in range(8):
                        P.mm(P.banks[bk].v(0), wbuf.v(k, dc * 128, (dc + 1) * 128), mix.v(k), start=(k == 0), stop=(k == 7))
                    P.stt(X.v(d, lo, lo + TT), P.banks[bk].v(0), gcol(d), X.v(d, lo, lo + TT), ALU.mult, ALU.add)
                    P.release(bk)

        def ffn_like(H, wg_d, wu_d, wd_d, F, gcol, WB, ws, nt=NT, ghook=None):
            ngr = (F + 3) // 4
            for g in range(ngr):
                gc = min(4, F - g * 4)
                slot = ws["i"] % 2
                ws["i"] += 1
                wgb, wub, wdb = ws["g"][slot], ws["u"][slot], ws["d"][slot]
                c0 = g * 512
                c1 = c0 + gc * 128
                P.dma(wgb.vc(0, 8, 0, gc * 128), wg_d.rearrange("(k p) n -> p k n", p=128)[:, :, c0:c1], q="pool")
                P.dma(wub.vc(0, 8, 0, gc * 128), wu_d.rearrange("(k p) n -> p k n", p=128)[:, :, c0:c1], q="pool")
                P.dma(wdb.vc(0, gc), wd_d[c0:c1, :].rearrange("(f p) d -> p f d", p=128), q="pool")
                if ghook is not None:
                    ghook(g)
                for tt in range(nt):
                    lo = tt * TT
                    actb = ws["act"].get()
                    for fc in range(gc):
                        bg = P.acquire()
                        bu = P.acquire()
                        for k in range(8):
                            P.mm(P.banks[bg].v(0), wgb.v(k, fc * 128, (fc + 1) * 128), H.v(k, lo, lo + TT), start=(k == 0), stop=(k == 7))
                        for k in range(8):
                            P.mm(P.banks[bu].v(0), wub.v(k, fc * 128, (fc + 1) * 128), H.v(k, lo, lo + TT), start=(k == 0), stop=(k == 7))
                        s = ws["s16"].get().v(0)
                        P.act(s, P.banks[bg].v(0), AF.Silu)
                        P.release(bg)
                        if WB is None:
                            P.tt(actb.v(fc), P.banks[bu].v(0), s, ALU.mult)
                        else:
                            u2 = ws["s32"].get().v(0)
                            P.tt(u2, P.banks[bu].v(0), WB.v(0, lo, lo + TT), ALU.mult)
                            P.tt(actb.v(fc), u2, s, ALU.mult)
                        P.release(bu)
                    ffn_flush(ws)
                    ws["pend"] = (wdb, actb, gc, lo, gcol)

        def ffn_flush(ws):
            if ws.get("pend") is None:
                return
            wdb, actb, gc, lo, gcol = ws["pend"]
            ws["pend"] = None
            for d in range(8):
                bk = P.acquire()
                for fc in range(gc):
                    P.mm(P.banks[bk].v(0), wdb.v(fc, d * 128, (d + 1) * 128), actb.v(fc), start=(fc == 0), stop=(fc == gc - 1))
                P.stt(X.v(d, lo, lo + TT), P.banks[bk].v(0), gcol(d), X.v(d, lo, lo + TT), ALU.mult, ALU.add)
                P.release(bk)

        def ffn_bufs(wb=False):
            return {"i": 0,
                    "g": [P.alloc(8, 512, BF16), P.alloc(8, 512, BF16)],
                    "u": [P.alloc(8, 512, BF16), P.alloc(8, 512, BF16)],
                    "d": [P.alloc(4, 1024, BF16), P.alloc(4, 1024, BF16)],
                    "act": RR([P.alloc(4, 512, BF16), P.alloc(4, 512, BF16)]),
                    "pend": None,
                    "s16": RR([P.alloc(1, 512, BF16) for _ in range(3)]),
                    "s32": RR([P.alloc(1, 512, F32) for _ in range(2)]) if wb else None}

        def l0_mix(seg):
            mk = P.mark()
            s32 = RR([P.alloc(1, TT, F32) for _ in range(10)])
            s16 = RR([P.alloc(1, TT, BF16) for _ in range(6)])
            Ht = P.alloc(8, TT, BF16)
            MIX = P.alloc(8, TT, BF16)
            YA = P.alloc(4, 30 + TT, BF16)
            BX = P.alloc(4, 3 + TT, BF16)
            WI = [P.alloc(8, 512, BF16), P.alloc(8, 512, BF16)]
            WO = P.alloc(8, 512, BF16)
            ACC = P.alloc(4, TT, F32)
            XB32 = P.alloc(4, TT, F32)
            wi = 0
            if seg == 0:
                for c in range(4):
                    P.memset(YAH.v(c), 0.0)
                    P.memset(BXH.v(c), 0.0)
                    P.memset(LST.v(c), 0.0)
            else:
                for c in range(4):
                    P.ts(YAH.v(c), YAH.v(c), FLG.v(0), ALU.mult)
                    P.ts(BXH.v(c), BXH.v(c), FLG.v(0), ALU.mult)
                    P.ts(LST.v(c), LST.v(c), FLG.v(0), ALU.mult)
            for t in range(NT):
                lo = t * TT
                norm_tile(lo, A1[0], lambda k: modc(0, 0, k), lambda k: Ht.v(k), s32, s16)
                hk = lambda k: Ht.v(k)
                for c in range(4):
                    P.copy(YA.v(c, 0, 30), YAH.v(c), eng="act")
                    P.copy(BX.v(c, 0, 3), BXH.v(c), eng="act")
                w = WI[wi % 2]; wi += 1
                stream_cols(e_w_in, 512, 1024, w)
                bks = proj_group(hk, w, 4)
                sg = []
                for c in range(4):
                    s = s16.get().v(0)
                    P.act(s, P.banks[bks[c]].v(0), AF.Sigmoid)
                    P.release(bks[c])
                    sg.append(s)
                w = WI[wi % 2]; wi += 1
                stream_cols(e_w_in, 0, 512, w)
                bks = proj_group(hk, w, 4)
                for c in range(4):
                    P.tt(YA.v(c, 30, 30 + TT), P.banks[bks[c]].v(0), sg[c], ALU.mult)
                    P.release(bks[c])
                    P.copy(YAH.v(c), YA.v(c, TT, TT + 30), eng="act")
                w = WI[wi % 2]; wi += 1
                stream_cols(e_w_in, 1024, 1536, w)
                bks = proj_group(hk, w, 4)
                for c in range(4):
                    P.act(BX.v(c, 3, 3 + TT), P.banks[bks[c]].v(0), AF.Identity)
                    P.release(bks[c])
                    P.copy(BXH.v(c), BX.v(c, TT, TT + 3), eng="act")
                NA = 15
                cbk = [P.acquire() for _ in range(4)]
                for j in range(31):
                    for c in range(4):
                        wcol = pe_col("caw", c * 31 + j)
                        if j < NA:
                            tmpj = s16.get().v(0)
                            P.act(tmpj, YA.v(c, j, j + TT), AF.Identity, scale=wcol)
                            P.mm(P.banks[cbk[c]].v(0), identb, tmpj, start=(j == 0), stop=(j == NA - 1))
                        elif j == NA:
                            P.ts(ACC.v(c), YA.v(c, j, j + TT), wcol, ALU.mult, pe_col("cab", c), ALU.add)
                        else:
                            P.stt(ACC.v(c), YA.v(c, j, j + TT), wcol, ACC.v(c), ALU.mult, ALU.add)
                for c in range(4):
                    P.tt(ACC.v(c), ACC.v(c), P.banks[cbk[c]].v(0), ALU.add)
                    P.release(cbk[c])
                for j in range(4):
                    for c in range(4):
                        wcol = pe_col("cbw", c * 4 + j)
                        if j == 0:
                            P.ts(XB32.v(c), BX.v(c, 0, TT), wcol, ALU.mult, pe_col("cbb", c), ALU.add)
                        else:
                            P.stt(XB32.v(c), BX.v(c, j, j + TT), wcol, XB32.v(c), ALU.mult, ALU.add)
                bm = P.acquire()
                bq = P.acquire()
                for c in range(4):
                    yb = s16.get().v(0)
                    ysq = s16.get().v(0)
                    P.act(yb, ACC.v(c), AF.Identity)
                    P.act(ysq, ACC.v(c), AF.Square)
                    P.mm(P.banks[bm].v(0), ones512, yb, start=(c == 0), stop=(c == 3))
                    P.mm(P.banks[bq].v(0), ones512, ysq, start=(c == 0), stop=(c == 3))
                Msb = s32.get().v(0)
                P.act(Msb, P.banks[bm].v(0), AF.Identity)
                P.release(bm)
                m2 = s32.get().v(0)
                P.tt(m2, Msb, Msb, ALU.mult)
                var = s32.get().v(0)
                P.tt(var, P.banks[bq].v(0), m2, ALU.subtract)
                P.release(bq)
                P.ts(var, var, 0.0, ALU.max)
                rstd_from_bank(var, var)
                for c in range(4):
                    tq = s32.get().v(0)
                    P.tt(tq, ACC.v(c), Msb, ALU.subtract)
                    P.tt(tq, tq, var, ALU.mult)
                    P.act(MIX.v(c), tq, AF.Silu, scale=pe_col("lng", c), bias=pe_col("lnb", c))
                w = WI[wi % 2]; wi += 1
                stream_cols(e_w_in, 1536, 2048, w)
                bgs = proj_group(hk, w, 4)
                for c in range(4):
                    xbb = s16.get().v(0)
                    P.act(xbb, XB32.v(c), AF.Identity)
                    br = P.acquire()
                    bi = P.acquire()
                    P.mm(P.banks[br].v(0), LRUW.v(c), xbb)
                    P.mm(P.banks[bi].v(0), LRUW.v(4 + c), xbb)
                    r = s32.get().v(0)
                    ii = s32.get().v(0)
                    P.act(r, P.banks[br].v(0), AF.Sigmoid, bias=pe_col("lba", c))
                    P.act(ii, P.banks[bi].v(0), AF.Sigmoid, bias=pe_col("lbx", c))
                    P.release(br)
                    P.release(bi)
                    a = s32.get().v(0)
                    a2 = s32.get().v(0)
                    P.act(a, r, AF.Exp, scale=der(32 + c))
                    P.act(a2, r, AF.Exp, scale=der(36 + c))
                    P.ts(a2, a2, -1.0, ALU.mult, 1.0, ALU.add)
                    P.ts(a2, a2, 0.0, ALU.max)
                    P.act(a2, a2, AF.Sqrt)
                    P.tt(ii, ii, XB32.v(c), ALU.mult)
                    P.tt(ii, ii, a2, ALU.mult)
                    hl = s32.get().v(0)
                    P.scan(hl, a, ii, LST.v(c))
                    P.copy(LST.v(c), V(hl.ap[:, TT - 1:TT], hl.bufs))
                    gl = s32.get().v(0)
                    P.act(gl, P.banks[bgs[c]].v(0), AF.Gelu_apprx_tanh)
                    P.release(bgs[c])
                    P.tt(MIX.v(4 + c), gl, hl, ALU.mult)
                out_proj(e_w_out, MIX, lambda d: modc(0, 2, d), lo, WO)
            P.reset(mk)

        def l0_ffn():
            mk = P.mark()
            H = P.alloc(8, T, BF16, tile=TT)
            mk2 = P.mark()
            s32 = RR([P.alloc(1, TT, F32) for _ in range(3)])
            s16 = RR([P.alloc(1, TT, BF16) for _ in range(3)])
            for t in range(NT):
                norm_tile(t * TT, A2[0], lambda k: modc(0, 3, k), lambda k, t=t: H.v(k, t * TT, (t + 1) * TT), s32, s16)
            P.reset(mk2)
            ws = ffn_bufs()
            ffn_like(H, e_wg, e_wu, e_wd, 22, lambda d: modc(0, 5, d), None, ws)
            ffn_flush(ws)
            P.reset(mk)

        def qk_finish(bks, dst, wn_col, slot0, s32, s16, cosT, sinT):
            for c in range(4):
                raw = s32.get().v(0)
                P.act(raw, P.banks[bks[c]].v(0), AF.Identity)
                P.release(bks[c])
                sq = s16.get().v(0)
                P.act(sq, raw, AF.Square)
                qw = s16.get().v(0)
                P.ts(qw, raw, wn_col, ALU.mult)
                bms = P.acquire()
                brt = P.acquire()
                P.mm(P.banks[bms].v(0), blk64, sq)
                P.mm(P.banks[brt].v(0), rperm, qw)
                R = s32.get().v(0)
                rstd_from_bank(P.banks[bms].v(0), R)
                P.release(bms)
                t1 = s32.get().v(0)
                P.tt(t1, qw, cosT, ALU.mult)
                t2 = raw
                P.tt(t2, P.banks[brt].v(0), sinT, ALU.mult)
                P.release(brt)
                P.tt(t1, t1, t2, ALU.add)
                P.tt(dst(c), t1, R, ALU.mult)

        def kv_tile(hk, slot_tile, KTs, VVs, WI, wstate, s32, s16, cosT, sinT, part="kv"):
            lo = slot_tile * TT
            if "k" in part:
                w = WI[wstate[0] % len(WI)]; wstate[0] += 1
                stream_cols(o_w_in, 2048, 2560, w)
                bks = proj_group(hk, w, 4)
                qk_finish(bks, lambda c: KTs.v(c, lo, lo + TT), po_col("kn"), None, s32, s16, cosT, sinT)
            if "v" not in part:
                return
            w = WI[wstate[0] % len(WI)]; wstate[0] += 1
            stream_cols(o_w_in, 2560, 3072, w)
            for s in range(4):
                bk = P.acquire()
                for k in range(8):
                    hv = hk(k)
                    P.mm(P.banks[bk].v(0), V(hv.ap[:, s * 128:(s + 1) * 128], hv.bufs), w.v(k), start=(k == 0), stop=(k == 7))
                ch_ = slot_tile * 4 + s
                vdst = V(VVs.full[:, ch_ * 520:(ch_ + 1) * 520].rearrange("p (h e) -> p h e", h=8)[:, :, 0:64], VVs.v(ch_).bufs)
                vsrc = V(P.banks[bk].full[:, 0:512].rearrange("p (h e) -> p h e", h=8), P.banks[bk].allbufs())
                P.act(vdst, vsrc, AF.Identity)
                P.release(bk)

        def kmean_tile(KTs, seg, slot_tile):
            for c in range(4):
                for hb in range(2):
                    blk = seg * 8 + slot_tile * 2 + hb
                    lo = slot_tile * TT + hb * 256
                    P.reduce(KMT.v(c, blk, blk + 1), KTs.v(c, lo, lo + 256), ALU.add)
                    P.act(KMTB.v(c, blk, blk + 1), KMT.v(c, blk, blk + 1), AF.Identity, scale=1.0 / 256.0)
                    for par in range(2):
                        P.ts(KMH[par].v(c, blk, blk + 1), KMT.v(c, blk, blk + 1), hmask[par], ALU.mult, 1.0 / 256.0, ALU.mult)

        def load_cs(seg, t, COS, SIN):
            s0 = seg * T + t * TT
            P.dma(COS.all(), cos_d[:, s0:s0 + TT])
            P.dma(SIN.all(), sin_d[:, s0:s0 + TT])

        def l1_kv_A(KTA, VVA):
            mk = P.mark()
            s32 = RR([P.alloc(1, TT, F32) for _ in range(6)])
            s16 = RR([P.alloc(1, TT, BF16) for _ in range(4)])
            Ht = P.alloc(8, TT, BF16)
            WI = [P.alloc(8, 512, BF16), P.alloc(8, 512, BF16)]
            COS = P.alloc(1, TT, F32)
            SIN = P.alloc(1, TT, F32)
            wst = [0]
            for t in range(NT):
                load_cs(0, t, COS, SIN)
                norm_tile(t * TT, A1[1], lambda k: modc(1, 0, k), lambda k: Ht.v(k), s32, s16)
                kv_tile(lambda k: Ht.v(k), t, KTA, VVA, WI, wst, s32, s16, COS.v(0), SIN.v(0))
                kmean_tile(KTA, 0, t)
                if t == NT - 1:
                    hk = lambda k: V(Ht.v(k).ap[:, TT - 128:TT], Ht.v(k).bufs)
                    w = WI[wst[0] % 2]; wst[0] += 1
                    stream_cols(o_w_in, 0, 512, w)
                    bks = proj_group(hk, w, 4, N=128)
                    chs = []
                    for c in range(4):
                        sx = s16.get().v(0)
                        P.act(V(sx.ap[:, 0:128], sx.bufs), P.banks[bks[c]].v(0, 0, 128), AF.Identity)
                        P.release(bks[c])
                        chs.append(sx)
                    w = WI[wst[0] % 2]; wst[0] += 1
                    stream_cols(o_w_in, 1024, 1536, w)
                    bks = proj_group(hk, w, 4, N=128)
                    for c in range(4):
                        P.tt(ZH.v(c), P.banks[bks[c]].v(0, 126, 128), V(chs[c].ap[:, 126:128], chs[c].bufs), ALU.mult)
                        P.release(bks[c])
                        P.ts(ZH.v(c), ZH.v(c), FLG.v(0), ALU.mult)
            P.reset(mk)

        def l1_mix_B(KTA, VVA, KTB, VVB):
            mk = P.mark()
            s32 = RR([P.alloc(1, TT, F32) for _ in range(3)])
            s16 = RR([P.alloc(1, TT, BF16) for _ in range(5)])
            Ht = P.alloc(8, TT, BF16)
            MIX = P.alloc(8, TT, BF16)
            QT = P.alloc(4, TT, BF16)
            Z = P.alloc(4, 2 + TT, BF16)
            WI = [P.alloc(8, 512, BF16), P.alloc(8, 512, BF16)]
            wq = [0]

            def nextw():
                w_ = WI[wq[0] % 2]
                wq[0] += 1
                return w_
            COS = P.alloc(1, TT, F32)
            SIN = P.alloc(1, TT, F32)
            MT = [P.alloc(1, TT, BF16), P.alloc(1, TT, BF16)]
            QH = [P.alloc(1, TT, BF16), P.alloc(1, TT, BF16)]
            P.memset(MT[0].all(), 0.0)
            P.memset(MT[1].all(), 0.0)
            G = P.alloc(1, 128, F32)
            G2 = P.alloc(1, 128, F32)
            MB = P.alloc(1, 512, F32)
            M1 = P.alloc(1, 8, F32)
            RD = P.alloc(1, 4, F32)
            YD = V(Ht.full.bitcast(F32), Ht.allbufs())
            for t in range(NT):
                lo = t * TT
                load_cs(1, t, COS, SIN)
                norm_tile(lo, A1[1], lambda k: modc(1, 0, k), lambda k: Ht.v(k), s32, s16)
                hk = lambda k: Ht.v(k)
                for c in range(4):
                    P.copy(Z.v(c, 0, 2), ZH.v(c), eng="act")
                w = nextw()
                stream_cols(o_w_in, 0, 512, w)
                bks = proj_group(hk, w, 4)
                chs = []
                for c in range(4):
                    s = s16.get().v(0)
                    P.act(s, P.banks[bks[c]].v(0), AF.Identity)
                    P.release(bks[c])
                    chs.append(s)
                w = nextw()
                stream_cols(o_w_in, 1024, 1536, w)
                bks = proj_group(hk, w, 4)
                for c in range(4):
                    P.tt(Z.v(c, 2, 2 + TT), P.banks[bks[c]].v(0), chs[c], ALU.mult)
                    P.release(bks[c])
                    P.copy(ZH.v(c), Z.v(c, TT, TT + 2), eng="act")
                w = nextw()
                stream_cols(o_w_in, 512, 1024, w)
                bks = proj_group(hk, w, 4)
                for c in range(4):
                    cv = s32.get().v(0)
                    for j in range(3):
                        wcol = po_col("ccw", c * 3 + j)
                        if j == 0:
                            P.ts(cv, Z.v(c, 0, TT), wcol, ALU.mult)
                        else:
                            P.stt(cv, Z.v(c, j, j + TT), wcol, cv, ALU.mult, ALU.add)
                    P.tt(MIX.v(c), P.banks[bks[c]].v(0), cv, ALU.mult)
                    P.release(bks[c])
                w = nextw()
                stream_cols(o_w_in, 1536, 2048, w)
                bks = proj_group(hk, w, 4)
                qk_finish(bks, lambda c: QT.v(c), po_col("qn"), None, s32, s16, COS.v(0), SIN.v(0))
                kv_tile(hk, t, KTB, VVB, WI, wq, s32, s16, COS.v(0), SIN.v(0), part="k")
                kmean_tile(KTB, 1, t)
                bk = P.acquire()
                gb = P.banks[bk]
                for s in range(4):
                    for h in range(8):
                        c, pb = h // 2, (h % 2) * 64
                        col = s * 128 + h * 16
                        P.mm(gb.v(0, col, col + 16), QT.v(c, s * 128, (s + 1) * 128), KMH[h % 2].v(c, 0, 16))
                kv_tile(hk, t, KTB, VVB, WI, wq, s32, s16, COS.v(0), SIN.v(0), part="v")
                for s in range(4):
                    own = 8 + 2 * t + s // 2
                    def v3(Tn, j0, j1):
                        so = 0 if Tn.N == 128 else s * 128
                        ap = Tn.full[:, so:so + 128].rearrange("p (h j) -> p h j", h=8)[:, :, j0:j1]
                        return V(ap, Tn.allbufs())
                    def bb(j0, j1):
                        return V(BLKB.full.unsqueeze(1).to_broadcast([128, 8, 16])[:, :, j0:j1], BLKB.allbufs())
                    def m1b():
                        return V(M1.full.unsqueeze(2).to_broadcast([128, 8, own]), M1.allbufs())
                    P.memset(v3(MB, 0, 16), -BIG)
                    P.memset(v3(MB, own, own + 1), 0.0)
                    P.tt(v3(G, 0, own), v3(gb, 0, own), bb(0, own), ALU.add)
                    P.copy(v3(G2, 0, own), v3(G, 0, own))
                    for rep in range(3):
                        P.reduce(M1.all(), v3(G2, 0, own), ALU.max)
                        if rep < 2:
                            e = v3(MB, 0, own)
                            P.tt(e, v3(G2, 0, own), m1b(), ALU.is_equal)
                            P.stt(v3(G2, 0, own), e, -BIG, v3(G2, 0, own), ALU.mult, ALU.add)
                    e = v3(MB, 0, own)
                    P.tt(e, v3(G, 0, own), m1b(), ALU.is_ge)
                    P.ts(e, e, -1.0, ALU.add, BIG, ALU.mult)
                    P.tt(e, e, bb(0, own), ALU.add)
                P.release(bk)
                nkc = 16 + 4 * (t + 1)
                for h in range(8):
                    c, pb = h // 2, (h % 2) * 64
                    mt = MT[h % 2]
                    bk = P.acquire()
                    for s in range(4):
                        col = s * 128 + h * 16
                        P.transpose(P.banks[bk].v(0, s * 128, (s + 1) * 128, 0, 16), MB.v(0, col, col + 16), ident32)
                    P.act(mt.v(0, 0, TT, 0, 16), P.banks[bk].v(0, 0, TT, 0, 16), AF.Identity)
                    P.release(bk)
                    qh = QH[h % 2]
                    P.ts(qh.all(), QT.v(c), hmask[h % 2], ALU.mult)
                    abk = [P.acquire() for _ in range(4)]

                    def s_group(kc):
                        if kc < 16:
                            kt, kl = KTA, kc
                        else:
                            kt, kl = KTB, kc - 16
                        bs = P.acquire()
                        S = P.banks[bs].v(0)
                        diag = kc >= 16 + 4 * t
                        P.mm(S, kt.v(c, kl * 128, (kl + 1) * 128), qh.all(), start=True, stop=False)
                        P.mm(S, EJ.v(kc // 2), mt.all(), start=False, stop=not diag)
                        if diag:
                            ci = kc - 16 - 4 * t
                            P.mm(S, identb, CC.v(0, 384 - ci * 128, 384 - ci * 128 + TT), start=False, stop=True)
                        return bs

                    LOOK = 2
                    pend = {}
                    for kc in range(min(LOOK, nkc)):
                        pend[kc] = s_group(kc)
                    for kc in range(nkc):
                        bs = pend.pop(kc)
                        pt = s16.get().v(0)
                        P.act(pt, P.banks[bs].v(0), AF.Exp, scale=0.125)
                        P.release(bs)
                        if kc + LOOK < nkc:
                            pend[kc + LOOK] = s_group(kc + LOOK)
                        vv, kl = (VVA, kc) if kc < 16 else (VVB, kc - 16)
                        for sq_ in range(4):
                            P.mm(P.banks[abk[sq_]].v(0, 0, 65), V(pt.ap[:, sq_ * 128:(sq_ + 1) * 128], pt.bufs),
                                 vv.v(kl, h * 65, h * 65 + 65), start=(kc == 0), stop=(kc == nkc - 1))
                    for sq_ in range(4):
                        P.recip(RD.v(0, sq_, sq_ + 1), P.banks[abk[sq_]].v(0, 64, 65))
                        P.ts(V(YD.ap[:, sq_ * 512 + h * 64: sq_ * 512 + h * 64 + 64], YD.bufs),
                             P.banks[abk[sq_]].v(0, 0, 64), RD.v(0, sq_, sq_ + 1), ALU.mult)
                        P.release(abk[sq_])
                for c in range(4):
                    bk = P.acquire()
                    for sq_ in range(4):
                        P.transpose(P.banks[bk].v(0, sq_ * 128, (sq_ + 1) * 128),
                                    V(YD.ap[:, sq_ * 512 + c * 128: sq_ * 512 + (c + 1) * 128], YD.bufs), ident32)
                    P.act(MIX.v(4 + c), P.banks[bk].v(0), AF.Identity)
                    P.release(bk)
                out_proj(o_w_out, MIX, lambda d: modc(1, 2, d), lo, nextw)
            P.reset(mk)

        def moe_B():
            mk = P.mark()
            H = P.alloc(8, T, BF16, tile=TT)
            WBs = [P.alloc(1, T, F32, tile=TT), P.alloc(1, T, F32, tile=TT)]
            D16 = [P.alloc(1, 128, F32) for _ in range(16)]
            mk2 = P.mark()
            s32 = RR([P.alloc(1, TT, F32) for _ in range(3)])
            s16 = RR([P.alloc(1, TT, BF16) for _ in range(3)])
            L8 = P.alloc(1, 32, F32)
            L8b = P.alloc(1, 32, F32)
            SEL = P.alloc(1, 32, F32)
            M4 = P.alloc(1, 4, F32)
            for t in range(NT):
                lbk = []

                def h32cb(k, tv):
                    if k == 0:
                        for s in range(4):
                            lbk.append(P.acquire())
                    h32 = s32.get().v(0)
                    P.act(h32, tv, AF.Identity, scale=A2[1](k), bias=modc(1, 3, k))
                    for s in range(4):
                        P.mm(P.banks[lbk[s]].v(0, 0, 8), V(h32.ap[:, s * 128:(s + 1) * 128], h32.bufs), RW.v(k),
                             start=(k == 0), stop=(k == 7))
                norm_tile(t * TT, A2[1], lambda k: modc(1, 3, k), lambda k, t=t: H.v(k, t * TT, (t + 1) * TT), s32, s16, h32cb)

                def v3(Tn, n=8):
                    return V(Tn.full[:, 0:4 * n].rearrange("p (s e) -> p s e", s=4), Tn.allbufs())
                rb3 = V(POt.full[:, PO_OFF["rb"]:PO_OFF["rb"] + 8].unsqueeze(1).to_broadcast([128, 4, 8]), POt.allbufs())
                m4b = lambda: V(M4.full.unsqueeze(2).to_broadcast([128, 4, 8]), M4.allbufs())
                for s in range(4):
                    P.tt(L8.v(0, s * 8, s * 8 + 8), P.banks[lbk[s]].v(0, 0, 8), po_col("rb", 0, 8), ALU.add)
                    P.release(lbk[s])
                P.reduce(M4.all(), v3(L8), ALU.max)
                P.tt(v3(SEL), v3(L8), m4b(), ALU.is_equal)
                P.stt(v3(L8b), v3(SEL), -BIG, v3(L8), ALU.mult, ALU.add)
                P.tt(v3(L8), v3(L8), m4b(), ALU.subtract)
                P.act(L8.all(), L8.all(), AF.Exp)
                P.reduce(M4.all(), v3(L8b), ALU.max)
                P.tt(v3(L8b), v3(L8b), m4b(), ALU.is_ge)
                P.tt(v3(SEL), v3(SEL), v3(L8b), ALU.add)
                P.tt(v3(L8), v3(L8), v3(SEL), ALU.mult)
                P.reduce(M4.all(), v3(L8), ALU.add)
                P.recip(M4.all(), M4.all())
                P.tt(V(WTOK.full[:, t * 32:(t + 1) * 32].rearrange("p (s e) -> p s e", s=4), WTOK.allbufs()),
                     v3(L8), m4b(), ALU.mult)
            P.reset(mk2)
            ws = ffn_bufs(True)

            def wb_diag(e):
                for st in range(16):
                    P.ts(D16[st].v(0), ident32, WTOK.v(0, st * 8 + e, st * 8 + e + 1), ALU.mult)

            def wb_mm(e):
                for t in range(NT):
                    bk = P.acquire()
                    for s in range(4):
                        P.mm(P.banks[bk].v(0, s * 128, (s + 1) * 128), ones32, D16[t * 4 + s].v(0))
                    P.act(WBs[e % 2].v(0, t * TT, (t + 1) * TT), P.banks[bk].v(0), AF.Identity)
                    P.release(bk)

            for e in range(8):
                wb_diag(e)
                wb_mm(e)
                ffn_like(H, m_wg[e * 1024:(e + 1) * 1024, :], m_wu[e * 1024:(e + 1) * 1024, :],
                         m_wd[e * 3584:(e + 1) * 3584, :], 28, lambda d: modc(1, 5, d), WBs[e % 2], ws)
            ffn_flush(ws)
            P.reset(mk)

        xr = lambda xd: xd.rearrange("(k p) t -> p k t", p=128)
        for k in range(8):
            P.dma(X.vc(k, k + 1), xr(xA)[:, k:k + 1, :])
        KVTOP = ARENA_BYTES
        if stage >= 1:
            l0_mix(0)
        compute_mod(1)
        if stage >= 2:
            l0_ffn()
        KTA = VVA = KTB = VVB = None
        if stage >= 3:
            KTA = P.alloc(4, T, BF16, tile=256, at=KVTOP - 16384)
            VVA = P.alloc(16, 520, BF16, at=KVTOP - 16384 - 16640)
            P.memset(V(VVA.full.rearrange("p (g e) -> p g e", e=65)[:, :, 64:65], VVA.allbufs()), 1.0)
            l1_kv_A(KTA, VVA)
        for k in range(8):
            P.dma(X.vc(k, k + 1), xr(xB)[:, k:k + 1, :])
        if stage >= 1:
            l0_mix(1)
        if stage >= 2:
            l0_ffn()
        if stage >= 4:
            KTB = P.alloc(4, T, BF16, tile=256, at=KVTOP - 33024 - 16384)
            VVB = P.alloc(16, 520, BF16, at=KVTOP - 33024 - 16384 - 16640)
            P.memset(V(VVB.full.rearrange("p (g e) -> p g e", e=65)[:, :, 64:65], VVB.allbufs()), 1.0)
            l1_mix_B(KTA, VVA, KTB, VVB)
            for tns in (KTA, VVA, KTB, VVB):
                P.free(tns)
        if stage >= 5:
            moe_B()
        outs = []
        for k in range(8):
            outs.append(P.dma(xr(out_d)[:, k:k + 1, :], X.vc(k, k + 1)))
        P.finish(outs)
        P.emit()
    return nc
from concourse.bass_utils import run_bass_kernel_spmd

ROPE_THETA = 10000.0
STAGE = 99
_NC_CACHE = {}


def _cols(v, n):
    return np.ascontiguousarray(np.asarray(v, np.float32).reshape(n, 128).T)


def _const_arrays():
    ident = np.eye(128, dtype=np.float32)
    ones = np.ones((128, 128), np.float32)
    hm = np.zeros((128, 2), np.float32)
    hm[:64, 0] = 1.0
    hm[64:, 1] = 1.0
    c32 = np.concatenate([ident, ones, hm], axis=1)
    blk = np.zeros((128, 128), np.float32)
    blk[:64, :64] = 1.0 / 64
    blk[64:, 64:] = 1.0 / 64
    rperm = np.zeros((128, 128), np.float32)
    for m in range(128):
        p = 64 * (m // 64) + ((m % 64) + 32) % 64
        rperm[p, m] = 1.0
    cb = np.concatenate([ident, ones / 1024.0, ones / 512.0, blk, ones, rperm], axis=1)
    kk = np.arange(128)[:, None]
    uu = np.arange(896)[None, :] - 384
    cc = np.where(kk <= uu, 0.0, -BIG).astype(np.float32)
    ej = np.zeros((128, 16, 128), np.float32)
    for j in range(16):
        ej[j, j, :] = 1.0
    return c32, cb, cc, ej.reshape(128, 2048)


def _rope_tables(sh):
    half = 32
    inv = (np.float32(ROPE_THETA) ** (-np.arange(half, dtype=np.float32) / np.float32(half))).astype(np.float32)
    slots = np.arange(2 * T)
    if sh == 1:
        pos = slots
    else:
        pos = np.where(slots >= T, slots - T, 0)
    ang = pos.astype(np.float32)[None, :] * inv[:, None]
    cos = np.cos(ang).astype(np.float32)
    sin = np.sin(ang).astype(np.float32)
    i = np.arange(128) % 64
    cosT = cos[i % 32]
    sinT = np.where((i < 32)[:, None], -sin[i % 32], sin[i % 32])
    return np.ascontiguousarray(cosT, np.float32), np.ascontiguousarray(sinT, np.float32)


def _prep(inp):
    g = lambda n: np.asarray(inp[n], np.float32)
    x = g("x")
    c = g("c")
    caw = g("e_conv_a_w")[0]
    cbw = g("e_conv_b_w")[0]
    ccw = g("o_conv_c_w")[0]
    pe = np.concatenate([
        _cols(g("e_norm_mix")[0], 8), _cols(g("e_norm_ffn")[0], 8), _cols(g("e_ada_b")[0], 48),
        np.ascontiguousarray(caw.T.reshape(4, 128, 31).transpose(1, 0, 2)).reshape(128, 124),
        _cols(g("e_conv_a_b")[0], 4), _cols(g("e_ln_a_g")[0], 4), _cols(g("e_ln_a_b")[0], 4),
        np.ascontiguousarray(cbw.T.reshape(4, 128, 4).transpose(1, 0, 2)).reshape(128, 16),
        _cols(g("e_conv_b_b")[0], 4), _cols(g("e_lru_ba")[0], 4), _cols(g("e_lru_bx")[0], 4),
        _cols(g("e_lru_lambda")[0], 4)], axis=1)
    assert pe.shape[1] == NPE
    po = np.concatenate([
        _cols(g("o_norm_mix")[0], 8), _cols(g("o_norm_ffn")[0], 8), _cols(g("o_ada_b")[0], 48),
        np.ascontiguousarray(ccw.T.reshape(4, 128, 3).transpose(1, 0, 2)).reshape(128, 12),
        np.tile(g("o_q_norm")[0], 2)[:, None], np.tile(g("o_k_norm")[0], 2)[:, None],
        np.broadcast_to(g("o_router_b")[0][None, :], (128, 8))], axis=1)
    assert po.shape[1] == NPO
    lru = np.zeros((128, 8, 128), np.float32)
    wa = g("e_lru_wa")[0]
    wx = g("e_lru_wx")[0]
    for cch in range(4):
        for hh in range(2):
            lru[hh * 64:(hh + 1) * 64, cch, hh * 64:(hh + 1) * 64] = wa[2 * cch + hh]
            lru[hh * 64:(hh + 1) * 64, 4 + cch, hh * 64:(hh + 1) * 64] = wx[2 * cch + hh]
    c32, cb, cc, ej = _const_arrays()
    shared = {
        "pe": np.ascontiguousarray(pe), "po": np.ascontiguousarray(po),
        "c32": c32, "cb": cb, "cc": cc, "ej": ej,
        "e_ada_w": g("e_ada_w")[0], "o_ada_w": g("o_ada_w")[0],
        "e_w_in": g("e_w_in")[0], "e_w_out": g("e_w_out")[0], "lru_w": lru.reshape(128, 1024),
        "e_ffn_wg": g("e_ffn_wg")[0], "e_ffn_wu": g("e_ffn_wu")[0], "e_ffn_wd": g("e_ffn_wd")[0],
        "o_w_in": g("o_w_in")[0], "o_w_out": g("o_w_out")[0], "o_router_w": g("o_router_w")[0],
    }
    if STAGE >= 5:
        shared["o_moe_wg"] = g("o_moe_wg")[0].reshape(8 * 1024, 3584)
        shared["o_moe_wu"] = g("o_moe_wu")[0].reshape(8 * 1024, 3584)
        shared["o_moe_wd"] = g("o_moe_wd")[0].reshape(8 * 3584, 1024)
    tabs = [_rope_tables(0), _rope_tables(1)]
    maps = []
    for core in range(8):
        b, sh = core // 2, core % 2
        m = dict(shared)
        if sh == 1:
            m["xA"] = np.ascontiguousarray(x[b, :T].T)
        else:
            m["xA"] = np.zeros((1024, T), np.float32)
        m["xB"] = np.ascontiguousarray(x[b, sh * T:(sh + 1) * T].T)
        m["cvec"] = _cols(c[b], 8)
        m["flag"] = np.full((128, 1), float(sh), np.float32)
        bb = np.zeros((128, 16), np.float32)
        if sh == 0:
            bb[:, :8] = -BIG
        m["blkb"] = bb
        m["cos"], m["sin"] = tabs[sh]
        maps.append(m)
    return maps


def kernel(**inputs):
    maps = _prep(inputs)
    if STAGE not in _NC_CACHE:
        _NC_CACHE[STAGE] = build_program(STAGE)
    nc = _NC_CACHE[STAGE]
    res = run_bass_kernel_spmd(nc, maps, core_ids=list(range(8)))
    out = np.empty((4, 2 * T, 1024), np.float32)
    for core in range(8):
        b, sh = core // 2, core % 2
        out[b, sh * T:(sh + 1) * T, :] = np.asarray(res.results[core]["out"], np.float32).T
    return out
```

```python
import numpy as np
from contextlib import ExitStack
import concourse.bass as bass
import concourse.mybir as mybir

F32 = mybir.dt.float32
BF16 = mybir.dt.bfloat16
AF = mybir.ActivationFunctionType
ALU = mybir.AluOpType
AX = mybir.AxisListType

ENGS = ["pe", "act", "dve", "pool", "sp"]
EPOCH = 6000
NDSEM = 14
ARENA_BYTES = 207 * 1024


class Buf:
    __slots__ = ("w", "r")

    def __init__(self):
        self.w = None
        self.r = set()


class V:
    __slots__ = ("ap", "bufs")

    def __init__(self, ap, bufs):
        self.ap = ap
        self.bufs = bufs


class Tens:
    def __init__(self, full_ap, P, C, N, tile):
        self.P, self.C, self.N = P, C, N
        self.tile = tile
        nt = (N + tile - 1) // tile
        self.bufs = [[Buf() for _ in range(nt)] for _ in range(C)]
        self.full = full_ap

    def allbufs(self):
        return [b for bl in self.bufs for b in bl]

    def v(self, c, lo=0, hi=None, p0=0, p1=None):
        if hi is None:
            hi = self.N
        if p1 is None:
            p1 = self.P
        ap = self.full[p0:p1, c * self.N + lo: c * self.N + hi]
        bl = self.bufs[c][lo // self.tile: (hi - 1) // self.tile + 1]
        return V(ap, list(bl))

    def vc(self, c0, c1, lo=0, hi=None, p0=0, p1=None):
        if hi is None:
            hi = self.N
        if p1 is None:
            p1 = self.P
        ap = self.full[p0:p1, c0 * self.N: c1 * self.N].rearrange("p (c n) -> p c n", c=c1 - c0)[:, :, lo:hi]
        bl = []
        for c in range(c0, c1):
            bl += self.bufs[c][lo // self.tile: (hi - 1) // self.tile + 1]
        return V(ap, bl)

    def all(self):
        return V(self.full, self.allbufs())


def _compress(deps, ops):
    best = {}
    out = set()
    for (e, i) in deps:
        if ops[e][i]["dma"]:
            out.add((e, i))
        else:
            if e not in best or best[e] < i:
                best[e] = i
    for e, i in best.items():
        out.add((e, i))
    return out


class Prog:
    def __init__(self, nc, es):
        self.nc = nc
        self.es = es
        self.ops = {e: [] for e in ENGS}
        self.ndma = {"sp": 0, "pool": 0}
        self.arena = es.enter_context(nc.sbuf_tensor("arena", [128, ARENA_BYTES // 2], BF16))
        self.arena_ap = self.arena[:]
        self.top = 0
        self.live = []
        self.retired = []
        self.banks = []
        for i in range(8):
            h = es.enter_context(nc.psum_tensor(f"bank{i}", [128, 512], F32))
            self.banks.append(Tens(h[:], 128, 1, 512, 512))
        self.free_banks = list(range(8))

    def acquire(self):
        assert self.free_banks, "out of PSUM banks"
        i = self.free_banks.pop(0)
        return i

    def release(self, i):
        self.free_banks.append(i)

    def alloc(self, C, N, dtype, tile=None, P=128, at=None):
        esz = 4 if dtype == F32 else 2
        nbytes = C * N * esz
        nbytes_al = (nbytes + 31) // 32 * 32
        if at is None:
            lo = self.top
            self.top += nbytes_al
        else:
            lo = at
        hi = lo + nbytes_al
        assert hi <= ARENA_BYTES, f"arena overflow {hi}"
        for (l2, h2, _) in self.live:
            assert hi <= l2 or lo >= h2, f"overlap with live tensor [{l2},{h2}) vs [{lo},{hi})"
        ap = self.arena_ap[0:P, lo // 2: (lo + nbytes) // 2]
        if dtype == F32:
            ap = ap.bitcast(F32)
        t = Tens(ap, P, C, N, tile or N)
        seeds = set()
        keep = []
        for (l2, h2, t2) in self.retired:
            if hi <= l2 or lo >= h2:
                keep.append((l2, h2, t2))
                continue
            for b in t2.allbufs():
                if b.w is not None:
                    seeds.add(b.w)
                seeds |= b.r
            if not (lo <= l2 and h2 <= hi):
                keep.append((l2, h2, t2))
        self.retired = keep
        if seeds:
            seeds = _compress(seeds, self.ops)
            for b in t.allbufs():
                b.r = set(seeds)
        self.live.append((lo, hi, t))
        return t

    def mark(self):
        return (self.top, len(self.live))

    def reset(self, mark):
        top, n = mark
        for ent in self.live[n:]:
            self.retired.append(ent)
        self.live = self.live[:n]
        self.top = top

    def free(self, t):
        for k, ent in enumerate(self.live):
            if ent[2] is t:
                self.retired.append(ent)
                del self.live[k]
                return
        raise KeyError

    def op(self, eng, fn, reads=(), writes=(), dma=False):
        idx = len(self.ops[eng])
        deps = set()
        for v in reads:
            for b in v.bufs:
                if b.w is not None:
                    deps.add(b.w)
        for v in writes:
            for b in v.bufs:
                if b.w is not None:
                    deps.add(b.w)
                deps |= b.r
        me = (eng, idx)
        deps.discard(me)
        if eng == "pe":
            deps = {d for d in deps if d[0] != "pe"}
        deps = _compress(deps, self.ops)
        o = {"fn": fn, "deps": deps, "sig": False, "dma": dma}
        if dma:
            q = self.ndma[eng]
            self.ndma[eng] += 1
            o["dq"] = q
        self.ops[eng].append(o)
        for v in reads:
            for b in v.bufs:
                if not dma:
                    b.r = {x for x in b.r if x[0] != eng or self.ops[eng][x[1]]["dma"]}
                b.r.add(me)
        for v in writes:
            for b in v.bufs:
                b.w = me
                b.r = set()
        return me

    def emit(self):
        nc = self.nc
        ops = self.ops
        for e in ENGS:
            for o in ops[e]:
                for (de, di) in o["deps"]:
                    ops[de][di]["sig"] = True
        nsig = {}
        for e in ENGS:
            r = 0
            for i, o in enumerate(ops[e]):
                if o["sig"] and not o["dma"]:
                    r += 1
                    o["rank"] = r
            nsig[e] = r
        es = self.es
        esem = {}
        for e in ENGS:
            n_ep = (nsig[e] + EPOCH - 1) // EPOCH
            esem[e] = [es.enter_context(nc.semaphore(f"s_{e}_{k}")) for k in range(max(n_ep, 1))]
        dsem = {q: [es.enter_context(nc.semaphore(f"d_{q}_{k}")) for k in range(NDSEM)] for q in ("sp", "pool")}
        block = es.enter_context(nc.Block())
        self.stats = {e: len(ops[e]) for e in ENGS}
        self.stats["nsig"] = nsig

        def run(eng, e):
            waited_rank = {x: 0 for x in ENGS}
            waited_dma = {}
            for o in ops[eng]:
                for (de, di) in sorted(o["deps"]):
                    d = ops[de][di]
                    if d["dma"]:
                        q = d["dq"]
                        k = q % NDSEM
                        val = 16 * (q // NDSEM + 1)
                        if waited_dma.get((de, k), 0) < val:
                            e.wait_ge(dsem[de][k], val)
                            waited_dma[(de, k)] = val
                    else:
                        r = d["rank"]
                        if waited_rank[de] < r:
                            ep = (r - 1) // EPOCH
                            e.wait_ge(esem[de][ep], r - ep * EPOCH)
                            waited_rank[de] = r
                if o["dma"]:
                    q = o["dq"]
                    k = q % NDSEM
                    prev = 16 * (q // NDSEM)
                    if prev > 0 and waited_dma.get((eng, k), 0) < prev:
                        e.wait_ge(dsem[eng][k], prev)
                        waited_dma[(eng, k)] = prev
                    ins = o["fn"](e)
                    ins.then_inc(dsem[eng][k], 16)
                elif o["fn"] is not None:
                    ins = o["fn"](e)
                    if o["sig"]:
                        r = o["rank"]
                        ep = (r - 1) // EPOCH
                        ins.then_inc(esem[eng][ep], 1)

        @block.tensor
        def _(e):
            run("pe", e)

        @block.scalar
        def _(e):
            run("act", e)

        @block.vector
        def _(e):
            run("dve", e)

        @block.gpsimd
        def _(e):
            run("pool", e)

        @block.sync
        def _(e):
            run("sp", e)

    def mm(self, out, lhsT, rhs, start=True, stop=True):
        return self.op("pe", lambda e: e.matmul(out.ap, lhsT.ap, rhs.ap, start=start, stop=stop),
                       reads=[lhsT, rhs], writes=[out])

    def transpose(self, out, in_, ident):
        return self.op("pe", lambda e: e.transpose(out.ap, in_.ap, ident.ap), reads=[in_, ident], writes=[out])

    def act(self, out, in_, func, scale=1.0, bias=0.0):
        reads = [in_]
        sc = scale
        bi = bias
        if isinstance(scale, V):
            reads.append(scale)
            sc = scale.ap
        if isinstance(bias, V):
            reads.append(bias)
            bi = bias.ap
        return self.op("act", lambda e: e.activation(out.ap, in_.ap, func, bias=bi, scale=sc),
                       reads=reads, writes=[out])

    def tt(self, out, a, b, op, eng="dve"):
        return self.op(eng, lambda e: e.tensor_tensor(out.ap, a.ap, b.ap, op), reads=[a, b], writes=[out])

    def ts(self, out, a, s1, op0, s2=None, op1=None, eng="dve"):
        reads = [a]
        x1 = s1
        x2 = s2
        if isinstance(s1, V):
            reads.append(s1)
            x1 = s1.ap
        if isinstance(s2, V):
            reads.append(s2)
            x2 = s2.ap
        if op1 is None:
            return self.op(eng, lambda e: e.tensor_scalar(out.ap, a.ap, x1, None, op0), reads=reads, writes=[out])
        return self.op(eng, lambda e: e.tensor_scalar(out.ap, a.ap, x1, x2, op0, op1), reads=reads, writes=[out])

    def stt(self, out, a, s, b, op0, op1):
        reads = [a, b]
        x = s
        if isinstance(s, V):
            reads.append(s)
            x = s.ap
        return self.op("dve", lambda e: e.scalar_tensor_tensor(out.ap, a.ap, x, b.ap, op0, op1), reads=reads, writes=[out])

    def copy(self, out, a, eng="dve"):
        if eng == "act":
            return self.act(out, a, AF.Identity)
        return self.op(eng, lambda e: e.tensor_copy(out.ap, a.ap), reads=[a], writes=[out])

    def memset(self, out, val, eng="dve"):
        return self.op(eng, lambda e: e.memset(out.ap, val), reads=[], writes=[out])

    def reduce(self, out, a, op, eng="dve"):
        return self.op(eng, lambda e: e.tensor_reduce(out.ap, a.ap, AX.X, op), reads=[a], writes=[out])

    def recip(self, out, a):
        return self.op("dve", lambda e: e.reciprocal(out.ap, a.ap), reads=[a], writes=[out])

    def scan(self, out, d0, d1, init):
        reads = [d0, d1]
        x = init
        if isinstance(init, V):
            reads.append(init)
            x = init.ap
        return self.op("dve", lambda e: e.tensor_tensor_scan(out.ap, d0.ap, d1.ap, x, ALU.mult, ALU.add),
                       reads=reads, writes=[out])

    def dma(self, out, in_, q="sp"):
        reads = [in_] if isinstance(in_, V) else []
        writes = [out] if isinstance(out, V) else []
        oa = out.ap if isinstance(out, V) else out
        ia = in_.ap if isinstance(in_, V) else in_
        return self.op(q, lambda e: e.dma_start(out=oa, in_=ia), reads=reads, writes=writes, dma=True)

    def finish(self, out_dma_ops):
        deps = set(out_dma_ops)
        self.ops["sp"].append({"fn": None, "deps": deps, "sig": False, "dma": False})

T = 2048
TT = 512
NT = T // TT
BIG = 30000.0
EPS = 1e-6

PE_OFF = {}
_o = 0
for _n, _w in [("nm", 8), ("nf", 8), ("adab", 48), ("caw", 124), ("cab", 4), ("lng", 4), ("lnb", 4),
               ("cbw", 16), ("cbb", 4), ("lba", 4), ("lbx", 4), ("lam", 4)]:
    PE_OFF[_n] = _o
    _o += _w
NPE = _o
PO_OFF = {}
_o = 0
for _n, _w in [("nm", 8), ("nf", 8), ("adab", 48), ("ccw", 12), ("qn", 1), ("kn", 1), ("rb", 8)]:
    PO_OFF[_n] = _o
    _o += _w
NPO = _o


class RR:
    def __init__(self, tiles):
        self.t = tiles
        self.i = 0

    def get(self):
        x = self.t[self.i % len(self.t)]
        self.i += 1
        return x


def build_program(stage=99):
    nc = bass.Bass("TRN2", target_bir_lowering=False)

    def din(name, shape):
        return nc.dram_tensor(name, shape, F32, kind="ExternalInput").ap()

    xA = din("xA", [1024, T])
    xB = din("xB", [1024, T])
    cvec = din("cvec", [128, 8])
    flag_d = din("flag", [128, 1])
    pe_d = din("pe", [128, NPE])
    po_d = din("po", [128, NPO])
    c32_d = din("c32", [128, 258])
    cb_d = din("cb", [128, 6 * 128])
    cc_d = din("cc", [128, 896])
    ej_d = din("ej", [128, 2048])
    blkb_d = din("blkb", [128, 16])
    cos_d = din("cos", [128, 2 * T])
    sin_d = din("sin", [128, 2 * T])
    e_ada_w = din("e_ada_w", [1024, 6144])
    o_ada_w = din("o_ada_w", [1024, 6144])
    e_w_in = din("e_w_in", [1024, 2048])
    e_w_out = din("e_w_out", [1024, 1024])
    lru_w = din("lru_w", [128, 8 * 128])
    e_wg = din("e_ffn_wg", [1024, 2816])
    e_wu = din("e_ffn_wu", [1024, 2816])
    e_wd = din("e_ffn_wd", [2816, 1024])
    o_w_in = din("o_w_in", [1024, 3072])
    o_w_out = din("o_w_out", [1024, 1024])
    o_rw = din("o_router_w", [1024, 8])
    if stage >= 5:
        m_wg = din("o_moe_wg", [8 * 1024, 3584])
        m_wu = din("o_moe_wu", [8 * 1024, 3584])
        m_wd = din("o_moe_wd", [8 * 3584, 1024])
    out_d = nc.dram_tensor("out", [1024, T], F32, kind="ExternalOutput").ap()

    es = ExitStack()
    with es:
        P = Prog(nc, es)
        X = P.alloc(8, T, F32, tile=TT)
        CB = P.alloc(6, 128, BF16)
        CC = P.alloc(1, 896, BF16)
        EJ = P.alloc(16, 128, BF16)
        C32 = P.alloc(1, 258, F32)
        PEt = P.alloc(1, NPE, F32)
        POt = P.alloc(1, NPO, F32)
        FLG = P.alloc(1, 1, F32)
        BLKB = P.alloc(1, 16, F32)
        CACT = P.alloc(1, 8, BF16)
        MOD = [P.alloc(1, 48, F32), P.alloc(1, 48, F32)]
        DER = P.alloc(1, 64, F32)
        LST = P.alloc(4, 1, F32)
        YAH = P.alloc(4, 30, BF16)
        BXH = P.alloc(4, 3, BF16)
        ZH = P.alloc(4, 2, BF16)
        KMT = P.alloc(4, 16, F32)
        KMTB = P.alloc(4, 16, BF16)
        KMH = [P.alloc(4, 16, BF16), P.alloc(4, 16, BF16)]
        WTOK = P.alloc(1, 128, F32)
        LRUW = P.alloc(8, 128, BF16)
        RW = P.alloc(8, 8, F32)

        identb, ones1024, ones512, blk64, onesb, rperm = [CB.v(i) for i in range(6)]
        ident32, ones32 = C32.v(0, 0, 128), C32.v(0, 128, 256)
        hmask = [C32.v(0, 256, 257), C32.v(0, 257, 258)]

        def pe_col(name, j=0, n=1):
            o = PE_OFF[name] + j
            return PEt.v(0, o, o + n)

        def po_col(name, j=0, n=1):
            o = PO_OFF[name] + j
            return POt.v(0, o, o + n)

        def der(j, n=1):
            return DER.v(0, j, j + n)

        P.dma(CB.all(), cb_d, q="pool")
        P.dma(CC.all(), cc_d, q="pool")
        P.dma(EJ.all(), ej_d, q="pool")
        P.dma(LRUW.all(), lru_w, q="pool")
        P.dma(C32.all(), c32_d)
        P.dma(PEt.all(), pe_d)
        P.dma(POt.all(), po_d)
        P.dma(FLG.all(), flag_d)
        P.dma(BLKB.all(), blkb_d)
        P.dma(RW.vc(0, 8), o_rw.rearrange("(k p) e -> p k e", p=128))
        P.memset(KMTB.all(), 0.0)
        P.memset(KMH[0].all(), 0.0)
        P.memset(KMH[1].all(), 0.0)

        m0 = P.mark()
        CV = P.alloc(1, 8, F32)
        P.dma(CV.all(), cvec)
        P.act(CACT.all(), CV.all(), AF.Silu)
        def compute_mod(L):
            mkm = P.mark()
            WS = [P.alloc(8, 1024, BF16), P.alloc(8, 1024, BF16)]
            wi = 0
            adaw = [e_ada_w, o_ada_w][L]
            bk = P.acquire()
            bank = P.banks[bk]
            adaw_r = adaw.rearrange("(k p) n -> p k n", p=128)
            for v6 in range(6):
                w = WS[wi % 2]
                wi += 1
                P.dma(w.vc(0, 8), adaw_r[:, :, v6 * 1024:(v6 + 1) * 1024], q="pool")
                for mc in range(8):
                    col = v6 * 8 + mc
                    for k in range(8):
                        P.mm(bank.v(0, col, col + 1), w.v(k, mc * 128, (mc + 1) * 128), CACT.v(0, k, k + 1),
                             start=(k == 0), stop=(k == 7))
            par = PEt if L == 0 else POt
            off = (PE_OFF if L == 0 else PO_OFF)["adab"]
            P.tt(MOD[L].all(), bank.v(0, 0, 48), par.v(0, off, off + 48), ALU.add)
            P.release(bk)
            offm = (PE_OFF if L == 0 else PO_OFF)
            P.stt(der(L * 16, 8), MOD[L].v(0, 8, 16), 1.0, par.v(0, offm["nm"], offm["nm"] + 8), ALU.add, ALU.mult)
            P.stt(der(L * 16 + 8, 8), MOD[L].v(0, 32, 40), 1.0, par.v(0, offm["nf"], offm["nf"] + 8), ALU.add, ALU.mult)
            P.reset(mkm)

        compute_mod(0)
        lam = pe_col("lam", 0, 4)
        t_abs = der(40, 4)
        t_e = der(44, 4)
        t_mx = der(48, 4)
        P.ts(t_mx, lam, -1.0, ALU.mult)
        P.tt(t_abs, lam, t_mx, ALU.max)
        P.act(t_e, t_abs, AF.Exp, scale=-1.0)
        P.act(t_e, t_e, AF.Ln, bias=1.0)
        P.ts(t_mx, lam, -1.0, ALU.mult, 0.0, ALU.max)
        P.tt(t_e, t_e, t_mx, ALU.add)
        P.ts(der(32, 4), t_e, -8.0, ALU.mult)
        P.ts(der(36, 4), t_e, -16.0, ALU.mult)
        P.reset(m0)

        A1 = [lambda k, L=L: der(L * 16 + k) for L in range(2)]
        A2 = [lambda k, L=L: der(L * 16 + 8 + k) for L in range(2)]

        def modc(L, which, k):
            return MOD[L].v(0, which * 8 + k, which * 8 + k + 1)

        def rstd_from_bank(bank, outR):
            P.act(outR, bank, AF.Ln, bias=EPS)
            P.act(outR, outR, AF.Exp, scale=-0.5)

        RN = P.alloc(1, TT, F32)

        def norm_tile(lo, Acol, shcol, hout, s32, s16, h32cb=None):
            bk = P.acquire()
            bank = P.banks[bk].v(0)
            for k in range(8):
                sq = s16.get().v(0)
                P.act(sq, X.v(k, lo, lo + TT), AF.Square)
                P.mm(bank, ones1024, sq, start=(k == 0), stop=(k == 7))
            R = RN.v(0)
            rstd_from_bank(bank, R)
            P.release(bk)
            for k in range(8):
                t = s32.get().v(0)
                P.tt(t, X.v(k, lo, lo + TT), R, ALU.mult)
                P.act(hout(k), t, AF.Identity, scale=Acol(k), bias=shcol(k))
                if h32cb is not None:
                    h32cb(k, t)

        def stream_cols(w_d, c0, c1, dst):
            P.dma(dst.vc(0, 8, 0, c1 - c0), w_d.rearrange("(k p) n -> p k n", p=128)[:, :, c0:c1], q="pool")

        def proj_group(h, w, nchunk, N=TT):
            bks = []
            for c in range(nchunk):
                bk = P.acquire()
                for k in range(8):
                    P.mm(P.banks[bk].v(0, 0, N), w.v(k, c * 128, (c + 1) * 128), h(k), start=(k == 0), stop=(k == 7))
                bks.append(bk)
            return bks

        def out_proj(w_d, mix, gcol, lo, wbuf):
            wbuf_in = wbuf
            for half in range(2):
                wbuf = wbuf_in() if callable(wbuf_in) else wbuf_in
                stream_cols(w_d, half * 512, (half + 1) * 512, wbuf)
                for dc in range(4):
                    d = half * 4 + dc
                    bk = P.acquire()
                    for k in range(8):
                        P.mm(P.banks[bk].v(0), wbuf.v(k, dc * 128, (dc + 1) * 128), mix.v(k), start=(k == 0), stop=(k == 7))
                    P.stt(X.v(d, lo, lo + TT), P.banks[bk].v(0), gcol(d), X.v(d, lo, lo + TT), ALU.mult, ALU.add)
                    P.release(bk)

        def ffn_like(H, wg_d, wu_d, wd_d, F, gcol, WB, ws, nt=NT, ghook=None):
            ngr = (F + 3) // 4
            for g in range(ngr):
                gc = min(4, F - g * 4)
                slot = ws["i"] % 2
                ws["i"] += 1
                wgb, wub, wdb = ws["g"][slot], ws["u"][slot], ws["d"][slot]
                c0 = g * 512
                c1 = c0 + gc * 128
                P.dma(wgb.vc(0, 8, 0, gc * 128), wg_d.rearrange("(k p) n -> p k n", p=128)[:, :, c0:c1], q="pool")
                P.dma(wub.vc(0, 8, 0, gc * 128), wu_d.rearrange("(k p) n -> p k n", p=128)[:, :, c0:c1], q="pool")
                P.dma(wdb.vc(0, gc), wd_d[c0:c1, :].rearrange("(f p) d -> p f d", p=128), q="pool")
                if ghook is not None:
                    ghook(g)
                for tt in range(nt):
                    lo = tt * TT
                    actb = ws["act"].get()
                    for fc in range(gc):
                        bg = P.acquire()
                        bu = P.acquire()
                        for k in range(8):
                            P.mm(P.banks[bg].v(0), wgb.v(k, fc * 128, (fc + 1) * 128), H.v(k, lo, lo + TT), start=(k == 0), stop=(k == 7))
                        for k in range(8):
                            P.mm(P.banks[bu].v(0), wub.v(k, fc * 128, (fc + 1) * 128), H.v(k, lo, lo + TT), start=(k == 0), stop=(k == 7))
                        s = ws["s16"].get().v(0)
                        P.act(s, P.banks[bg].v(0), AF.Silu)
                        P.release(bg)
                        if WB is None:
                            P.tt(actb.v(fc), P.banks[bu].v(0), s, ALU.mult)
                        else:
                            u2 = ws["s32"].get().v(0)
                            P.tt(u2, P.banks[bu].v(0), WB.v(0, lo, lo + TT), ALU.mult)
                            P.tt(actb.v(fc), u2, s, ALU.mult)
                        P.release(bu)
                    ffn_flush(ws)
                    ws["pend"] = (wdb, actb, gc, lo, gcol)

        def ffn_flush(ws):
            if ws.get("pend") is None:
                return
            wdb, actb, gc, lo, gcol = ws["pend"]
            ws["pend"] = None
            for d in range(8):
                bk = P.acquire()
                for fc in range(gc):
                    P.mm(P.banks[bk].v(0), wdb.v(fc, d * 128, (d + 1) * 128), actb.v(fc), start=(fc == 0), stop=(fc == gc - 1))
                P.stt(X.v(d, lo, lo + TT), P.banks[bk].v(0), gcol(d), X.v(d, lo, lo + TT), ALU.mult, ALU.add)
                P.release(bk)

        def ffn_bufs(wb=False):
            return {"i": 0,
                    "g": [P.alloc(8, 512, BF16), P.alloc(8, 512, BF16)],
                    "u": [P.alloc(8, 512, BF16), P.alloc(8, 512, BF16)],
                    "d": [P.alloc(4, 1024, BF16), P.alloc(4, 1024, BF16)],
                    "act": RR([P.alloc(4, 512, BF16), P.alloc(4, 512, BF16)]),
                    "pend": None,
                    "s16": RR([P.alloc(1, 512, BF16) for _ in range(3)]),
                    "s32": RR([P.alloc(1, 512, F32) for _ in range(2)]) if wb else None}

        def l0_mix(seg):
            mk = P.mark()
            s32 = RR([P.alloc(1, TT, F32) for _ in range(10)])
            s16 = RR([P.alloc(1, TT, BF16) for _ in range(6)])
            Ht = P.alloc(8, TT, BF16)
            MIX = P.alloc(8, TT, BF16)
            YA = P.alloc(4, 30 + TT, BF16)
            BX = P.alloc(4, 3 + TT, BF16)
            WI = [P.alloc(8, 512, BF16), P.alloc(8, 512, BF16)]
            ACC = P.alloc(4, TT, F32)
            XB32 = P.alloc(4, TT, F32)
            wi = 0
            if seg == 0:
                for c in range(4):
                    P.memset(YAH.v(c), 0.0)
                    P.memset(BXH.v(c), 0.0)
                    P.memset(LST.v(c), 0.0)
            else:
                for c in range(4):
                    P.ts(YAH.v(c), YAH.v(c), FLG.v(0), ALU.mult)
                    P.ts(BXH.v(c), BXH.v(c), FLG.v(0), ALU.mult)
                    P.ts(LST.v(c), LST.v(c), FLG.v(0), ALU.mult)
            for t in range(NT):
                lo = t * TT
                norm_tile(lo, A1[0], lambda k: modc(0, 0, k), lambda k: Ht.v(k), s32, s16)
                hk = lambda k: Ht.v(k)
                for c in range(4):
                    P.copy(YA.v(c, 0, 30), YAH.v(c), eng="act")
                    P.copy(BX.v(c, 0, 3), BXH.v(c), eng="act")
                w = WI[wi % 2]; wi += 1
                stream_cols(e_w_in, 512, 1024, w)
                bks = proj_group(hk, w, 4)
                sg = []
                for c in range(4):
                    s = s16.get().v(0)
                    P.act(s, P.banks[bks[c]].v(0), AF.Sigmoid)
                    P.release(bks[c])
                    sg.append(s)
                w = WI[wi % 2]; wi += 1
                stream_cols(e_w_in, 0, 512, w)
                bks = proj_group(hk, w, 4)
                for c in range(4):
                    P.tt(YA.v(c, 30, 30 + TT), P.banks[bks[c]].v(0), sg[c], ALU.mult)
                    P.release(bks[c])
                    P.copy(YAH.v(c), YA.v(c, TT, TT + 30), eng="act")
                w = WI[wi % 2]; wi += 1
                stream_cols(e_w_in, 1024, 1536, w)
                bks = proj_group(hk, w, 4)
                for c in range(4):
                    P.act(BX.v(c, 3, 3 + TT), P.banks[bks[c]].v(0), AF.Identity)
                    P.release(bks[c])
                    P.copy(BXH.v(c), BX.v(c, TT, TT + 3), eng="act")
                NA = 15
                cbk = [P.acquire() for _ in range(4)]
                for j in range(31):
                    for c in range(4):
                        wcol = pe_col("caw", c * 31 + j)
                        if j < NA:
                            tmpj = s16.get().v(0)
                            P.act(tmpj, YA.v(c, j, j + TT), AF.Identity, scale=wcol)
                            P.mm(P.banks[cbk[c]].v(0), identb, tmpj, start=(j == 0), stop=(j == NA - 1))
                        elif j == NA:
                            P.ts(ACC.v(c), YA.v(c, j, j + TT), wcol, ALU.mult, pe_col("cab", c), ALU.add)
                        else:
                            P.stt(ACC.v(c), YA.v(c, j, j + TT), wcol, ACC.v(c), ALU.mult, ALU.add)
                for c in range(4):
                    P.tt(ACC.v(c), ACC.v(c), P.banks[cbk[c]].v(0), ALU.add)
                    P.release(cbk[c])
                for j in range(4):
                    for c in range(4):
                        wcol = pe_col("cbw", c * 4 + j)
                        if j == 0:
                            P.ts(XB32.v(c), BX.v(c, 0, TT), wcol, ALU.mult, pe_col("cbb", c), ALU.add)
                        else:
                            P.stt(XB32.v(c), BX.v(c, j, j + TT), wcol, XB32.v(c), ALU.mult, ALU.add)
                bm = P.acquire()
                bq = P.acquire()
                for c in range(4):
                    yb = s16.get().v(0)
                    ysq = s16.get().v(0)
                    P.act(yb, ACC.v(c), AF.Identity)
                    P.act(ysq, ACC.v(c), AF.Square)
                    P.mm(P.banks[bm].v(0), ones512, yb, start=(c == 0), stop=(c == 3))
                    P.mm(P.banks[bq].v(0), ones512, ysq, start=(c == 0), stop=(c == 3))
                Msb = s32.get().v(0)
                P.act(Msb, P.banks[bm].v(0), AF.Identity)
                P.release(bm)
                m2 = s32.get().v(0)
                P.tt(m2, Msb, Msb, ALU.mult)
                var = s32.get().v(0)
                P.tt(var, P.banks[bq].v(0), m2, ALU.subtract)
                P.release(bq)
                P.ts(var, var, 0.0, ALU.max)
                rstd_from_bank(var, var)
                for c in range(4):
                    tq = s32.get().v(0)
                    P.tt(tq, ACC.v(c), Msb, ALU.subtract)
                    P.tt(tq, tq, var, ALU.mult)
                    P.act(MIX.v(c), tq, AF.Silu, scale=pe_col("lng", c), bias=pe_col("lnb", c))
                w = WI[wi % 2]; wi += 1
                stream_cols(e_w_in, 1536, 2048, w)
                bgs = proj_group(hk, w, 4)
                for c in range(4):
                    xbb = s16.get().v(0)
                    P.act(xbb, XB32.v(c), AF.Identity)
                    br = P.acquire()
                    bi = P.acquire()
                    P.mm(P.banks[br].v(0), LRUW.v(c), xbb)
                    P.mm(P.banks[bi].v(0), LRUW.v(4 + c), xbb)
                    r = s32.get().v(0)
                    ii = s32.get().v(0)
                    P.act(r, P.banks[br].v(0), AF.Sigmoid, bias=pe_col("lba", c))
                    P.act(ii, P.banks[bi].v(0), AF.Sigmoid, bias=pe_col("lbx", c))
                    P.release(br)
                    P.release(bi)
                    a = s32.get().v(0)
                    a2 = s32.get().v(0)
                    P.act(a, r, AF.Exp, scale=der(32 + c))
                    P.act(a2, r, AF.Exp, scale=der(36 + c))
                    P.ts(a2, a2, -1.0, ALU.mult, 1.0, ALU.add)
                    P.ts(a2, a2, 0.0, ALU.max)
                    P.act(a2, a2, AF.Sqrt)
                    P.tt(ii, ii, XB32.v(c), ALU.mult)
                    P.tt(ii, ii, a2, ALU.mult)
                    hl = s32.get().v(0)
                    P.scan(hl, a, ii, LST.v(c))
                    P.copy(LST.v(c), V(hl.ap[:, TT - 1:TT], hl.bufs))
                    gl = s32.get().v(0)
                    P.act(gl, P.banks[bgs[c]].v(0), AF.Gelu_apprx_tanh)
                    P.release(bgs[c])
                    P.tt(MIX.v(4 + c), gl, hl, ALU.mult)
                def nextw0():
                    nonlocal wi
                    w_ = WI[wi % 2]
                    wi += 1
                    return w_
                out_proj(e_w_out, MIX, lambda d: modc(0, 2, d), lo, nextw0)
            P.reset(mk)

        def l0_ffn():
            mk = P.mark()
            H = P.alloc(8, T, BF16, tile=TT)
            mk2 = P.mark()
            s32 = RR([P.alloc(1, TT, F32) for _ in range(3)])
            s16 = RR([P.alloc(1, TT, BF16) for _ in range(3)])
            for t in range(NT):
                norm_tile(t * TT, A2[0], lambda k: modc(0, 3, k), lambda k, t=t: H.v(k, t * TT, (t + 1) * TT), s32, s16)
            P.reset(mk2)
            ws = ffn_bufs()
            ffn_like(H, e_wg, e_wu, e_wd, 22, lambda d: modc(0, 5, d), None, ws)
            ffn_flush(ws)
            P.reset(mk)

        def qk_finish(bks, dst, wn_col, slot0, s32, s16, cosT, sinT):
            for c in range(4):
                raw = s32.get().v(0)
                P.act(raw, P.banks[bks[c]].v(0), AF.Identity)
                P.release(bks[c])
                sq = s16.get().v(0)
                P.act(sq, raw, AF.Square)
                qw = s16.get().v(0)
                P.ts(qw, raw, wn_col, ALU.mult)
                bms = P.acquire()
                brt = P.acquire()
                P.mm(P.banks[bms].v(0), blk64, sq)
                P.mm(P.banks[brt].v(0), rperm, qw)
                R = s32.get().v(0)
                rstd_from_bank(P.banks[bms].v(0), R)
                P.release(bms)
                t1 = s32.get().v(0)
                P.tt(t1, qw, cosT, ALU.mult)
                t2 = raw
                P.tt(t2, P.banks[brt].v(0), sinT, ALU.mult)
                P.release(brt)
                P.tt(t1, t1, t2, ALU.add)
                P.tt(dst(c), t1, R, ALU.mult)

        def kv_tile(hk, slot_tile, KTs, VVs, WI, wstate, s32, s16, cosT, sinT, part="kv"):
            lo = slot_tile * TT
            if "k" in part:
                w = WI[wstate[0] % len(WI)]; wstate[0] += 1
                stream_cols(o_w_in, 2048, 2560, w)
                bks = proj_group(hk, w, 4)
                qk_finish(bks, lambda c: KTs.v(c, lo, lo + TT), po_col("kn"), None, s32, s16, cosT, sinT)
            if "v" not in part:
                return
            w = WI[wstate[0] % len(WI)]; wstate[0] += 1
            stream_cols(o_w_in, 2560, 3072, w)
            for s in range(4):
                bk = P.acquire()
                for k in range(8):
                    hv = hk(k)
                    P.mm(P.banks[bk].v(0), V(hv.ap[:, s * 128:(s + 1) * 128], hv.bufs), w.v(k), start=(k == 0), stop=(k == 7))
                ch_ = slot_tile * 4 + s
                vdst = V(VVs.full[:, ch_ * 520:(ch_ + 1) * 520].rearrange("p (h e) -> p h e", h=8)[:, :, 0:64], VVs.v(ch_).bufs)
                vsrc = V(P.banks[bk].full[:, 0:512].rearrange("p (h e) -> p h e", h=8), P.banks[bk].allbufs())
                P.act(vdst, vsrc, AF.Identity)
                P.release(bk)

        def kmean_tile(KTs, seg, slot_tile):
            for c in range(4):
                for hb in range(2):
                    blk = seg * 8 + slot_tile * 2 + hb
                    lo = slot_tile * TT + hb * 256
                    P.reduce(KMT.v(c, blk, blk + 1), KTs.v(c, lo, lo + 256), ALU.add)
                    P.act(KMTB.v(c, blk, blk + 1), KMT.v(c, blk, blk + 1), AF.Identity, scale=1.0 / 256.0)
                    for par in range(2):
                        P.ts(KMH[par].v(c, blk, blk + 1), KMT.v(c, blk, blk + 1), hmask[par], ALU.mult, 1.0 / 256.0, ALU.mult)

        def load_cs(seg, t, COS, SIN):
            s0 = seg * T + t * TT
            P.dma(COS.all(), cos_d[:, s0:s0 + TT])
            P.dma(SIN.all(), sin_d[:, s0:s0 + TT])

        def l1_kv_A(KTA, VVA):
            mk = P.mark()
            s32 = RR([P.alloc(1, TT, F32) for _ in range(6)])
            s16 = RR([P.alloc(1, TT, BF16) for _ in range(4)])
            Ht = P.alloc(8, TT, BF16)
            WI = [P.alloc(8, 512, BF16), P.alloc(8, 512, BF16)]
            COS = P.alloc(1, TT, F32)
            SIN = P.alloc(1, TT, F32)
            wst = [0]
            for t in range(NT):
                load_cs(0, t, COS, SIN)
                norm_tile(t * TT, A1[1], lambda k: modc(1, 0, k), lambda k: Ht.v(k), s32, s16)
                kv_tile(lambda k: Ht.v(k), t, KTA, VVA, WI, wst, s32, s16, COS.v(0), SIN.v(0))
                kmean_tile(KTA, 0, t)
                if t == NT - 1:
                    hk = lambda k: V(Ht.v(k).ap[:, TT - 128:TT], Ht.v(k).bufs)
                    w = WI[wst[0] % 2]; wst[0] += 1
                    stream_cols(o_w_in, 0, 512, w)
                    bks = proj_group(hk, w, 4, N=128)
                    chs = []
                    for c in range(4):
                        sx = s16.get().v(0)
                        P.act(V(sx.ap[:, 0:128], sx.bufs), P.banks[bks[c]].v(0, 0, 128), AF.Identity)
                        P.release(bks[c])
                        chs.append(sx)
                    w = WI[wst[0] % 2]; wst[0] += 1
                    stream_cols(o_w_in, 1024, 1536, w)
                    bks = proj_group(hk, w, 4, N=128)
                    for c in range(4):
                        P.tt(ZH.v(c), P.banks[bks[c]].v(0, 126, 128), V(chs[c].ap[:, 126:128], chs[c].bufs), ALU.mult)
                        P.release(bks[c])
                        P.ts(ZH.v(c), ZH.v(c), FLG.v(0), ALU.mult)
            P.reset(mk)

        def l1_mix_B(KTA, VVA, KTB, VVB):
            mk = P.mark()
            s32 = RR([P.alloc(1, TT, F32) for _ in range(3)])
            s16 = RR([P.alloc(1, TT, BF16) for _ in range(5)])
            Ht = P.alloc(8, TT, BF16)
            MIX = P.alloc(8, TT, BF16)
            QT = P.alloc(4, TT, BF16)
            Z = P.alloc(4, 2 + TT, BF16)
            WI = [P.alloc(8, 512, BF16), P.alloc(8, 512, BF16)]
            wq = [0]

            def nextw():
                w_ = WI[wq[0] % 2]
                wq[0] += 1
                return w_
            COS = P.alloc(1, TT, F32)
            SIN = P.alloc(1, TT, F32)
            MT = [P.alloc(1, TT, BF16), P.alloc(1, TT, BF16)]
            QH = [P.alloc(1, TT, BF16), P.alloc(1, TT, BF16)]
            P.memset(MT[0].all(), 0.0)
            P.memset(MT[1].all(), 0.0)
            G = P.alloc(1, 128, F32)
            G2 = P.alloc(1, 128, F32)
            MB = P.alloc(1, 512, F32)
            M1 = P.alloc(1, 8, F32)
            RD = P.alloc(1, 4, F32)
            YD = V(Ht.full.bitcast(F32), Ht.allbufs())
            for t in range(NT):
                lo = t * TT
                load_cs(1, t, COS, SIN)
                norm_tile(lo, A1[1], lambda k: modc(1, 0, k), lambda k: Ht.v(k), s32, s16)
                hk = lambda k: Ht.v(k)
                for c in range(4):
                    P.copy(Z.v(c, 0, 2), ZH.v(c), eng="act")
                w = nextw()
                stream_cols(o_w_in, 0, 512, w)
                bks = proj_group(hk, w, 4)
                chs = []
                for c in range(4):
                    s = s16.get().v(0)
                    P.act(s, P.banks[bks[c]].v(0), AF.Identity)
                    P.release(bks[c])
                    chs.append(s)
                w = nextw()
                stream_cols(o_w_in, 1024, 1536, w)
                bks = proj_group(hk, w, 4)
                for c in range(4):
                    P.tt(Z.v(c, 2, 2 + TT), P.banks[bks[c]].v(0), chs[c], ALU.mult)
                    P.release(bks[c])
                    P.copy(ZH.v(c), Z.v(c, TT, TT + 2), eng="act")
                w = nextw()
                stream_cols(o_w_in, 512, 1024, w)
                bks = proj_group(hk, w, 4)
                for c in range(4):
                    cv = s32.get().v(0)
                    for j in range(3):
                        wcol = po_col("ccw", c * 3 + j)
                        if j == 0:
                            P.ts(cv, Z.v(c, 0, TT), wcol, ALU.mult)
                        else:
                            P.stt(cv, Z.v(c, j, j + TT), wcol, cv, ALU.mult, ALU.add)
                    P.tt(MIX.v(c), P.banks[bks[c]].v(0), cv, ALU.mult)
                    P.release(bks[c])
                w = nextw()
                stream_cols(o_w_in, 1536, 2048, w)
                bks = proj_group(hk, w, 4)
                qk_finish(bks, lambda c: QT.v(c), po_col("qn"), None, s32, s16, COS.v(0), SIN.v(0))
                kv_tile(hk, t, KTB, VVB, WI, wq, s32, s16, COS.v(0), SIN.v(0), part="k")
                kmean_tile(KTB, 1, t)
                bk = P.acquire()
                gb = P.banks[bk]
                for s in range(4):
                    for h in range(8):
                        c, pb = h // 2, (h % 2) * 64
                        col = s * 128 + h * 16
                        P.mm(gb.v(0, col, col + 16), QT.v(c, s * 128, (s + 1) * 128), KMH[h % 2].v(c, 0, 16))
                kv_tile(hk, t, KTB, VVB, WI, wq, s32, s16, COS.v(0), SIN.v(0), part="v")
                for s in range(4):
                    own = 8 + 2 * t + s // 2
                    def v3(Tn, j0, j1):
                        so = 0 if Tn.N == 128 else s * 128
                        ap = Tn.full[:, so:so + 128].rearrange("p (h j) -> p h j", h=8)[:, :, j0:j1]
                        return V(ap, Tn.allbufs())
                    def bb(j0, j1):
                        return V(BLKB.full.unsqueeze(1).to_broadcast([128, 8, 16])[:, :, j0:j1], BLKB.allbufs())
                    def m1b():
                        return V(M1.full.unsqueeze(2).to_broadcast([128, 8, own]), M1.allbufs())
                    P.memset(v3(MB, 0, 16), -BIG)
                    P.memset(v3(MB, own, own + 1), 0.0)
                    P.tt(v3(G, 0, own), v3(gb, 0, own), bb(0, own), ALU.add)
                    P.copy(v3(G2, 0, own), v3(G, 0, own))
                    for rep in range(3):
                        P.reduce(M1.all(), v3(G2, 0, own), ALU.max)
                        if rep < 2:
                            e = v3(MB, 0, own)
                            P.tt(e, v3(G2, 0, own), m1b(), ALU.is_equal)
                            P.stt(v3(G2, 0, own), e, -BIG, v3(G2, 0, own), ALU.mult, ALU.add)
                    e = v3(MB, 0, own)
                    P.tt(e, v3(G, 0, own), m1b(), ALU.is_ge)
                    P.ts(e, e, -1.0, ALU.add, BIG, ALU.mult)
                    P.tt(e, e, bb(0, own), ALU.add)
                P.release(bk)
                nkc = 16 + 4 * (t + 1)
                for h in range(8):
                    c, pb = h // 2, (h % 2) * 64
                    mt = MT[h % 2]
                    bk = P.acquire()
                    for s in range(4):
                        col = s * 128 + h * 16
                        P.transpose(P.banks[bk].v(0, s * 128, (s + 1) * 128, 0, 16), MB.v(0, col, col + 16), ident32)
                    P.act(mt.v(0, 0, TT, 0, 16), P.banks[bk].v(0, 0, TT, 0, 16), AF.Identity)
                    P.release(bk)
                    qh = QH[h % 2]
                    P.ts(qh.all(), QT.v(c), hmask[h % 2], ALU.mult)
                    abk = [P.acquire() for _ in range(4)]

                    def s_group(kc):
                        if kc < 16:
                            kt, kl = KTA, kc
                        else:
                            kt, kl = KTB, kc - 16
                        bs = P.acquire()
                        S = P.banks[bs].v(0)
                        diag = kc >= 16 + 4 * t
                        P.mm(S, kt.v(c, kl * 128, (kl + 1) * 128), qh.all(), start=True, stop=False)
                        P.mm(S, EJ.v(kc // 2), mt.all(), start=False, stop=not diag)
                        if diag:
                            ci = kc - 16 - 4 * t
                            P.mm(S, identb, CC.v(0, 384 - ci * 128, 384 - ci * 128 + TT), start=False, stop=True)
                        return bs

                    LOOK = 2
                    pend = {}
                    for kc in range(min(LOOK, nkc)):
                        pend[kc] = s_group(kc)
                    for kc in range(nkc):
                        bs = pend.pop(kc)
                        pt = s16.get().v(0)
                        P.act(pt, P.banks[bs].v(0), AF.Exp, scale=0.125)
                        P.release(bs)
                        if kc + LOOK < nkc:
                            pend[kc + LOOK] = s_group(kc + LOOK)
                        vv, kl = (VVA, kc) if kc < 16 else (VVB, kc - 16)
                        for sq_ in range(4):
                            P.mm(P.banks[abk[sq_]].v(0, 0, 65), V(pt.ap[:, sq_ * 128:(sq_ + 1) * 128], pt.bufs),
                                 vv.v(kl, h * 65, h * 65 + 65), start=(kc == 0), stop=(kc == nkc - 1))
                    for sq_ in range(4):
                        P.recip(RD.v(0, sq_, sq_ + 1), P.banks[abk[sq_]].v(0, 64, 65))
                        P.ts(V(YD.ap[:, sq_ * 512 + h * 64: sq_ * 512 + h * 64 + 64], YD.bufs),
                             P.banks[abk[sq_]].v(0, 0, 64), RD.v(0, sq_, sq_ + 1), ALU.mult)
                        P.release(abk[sq_])
                for c in range(4):
                    bk = P.acquire()
                    for sq_ in range(4):
                        P.transpose(P.banks[bk].v(0, sq_ * 128, (sq_ + 1) * 128),
                                    V(YD.ap[:, sq_ * 512 + c * 128: sq_ * 512 + (c + 1) * 128], YD.bufs), ident32)
                    P.act(MIX.v(4 + c), P.banks[bk].v(0), AF.Identity)
                    P.release(bk)
                out_proj(o_w_out, MIX, lambda d: modc(1, 2, d), lo, nextw)
            P.reset(mk)

        def moe_B():
            mk = P.mark()
            H = P.alloc(8, T, BF16, tile=TT)
            WBs = [P.alloc(1, T, F32, tile=TT), P.alloc(1, T, F32, tile=TT)]
            D16 = [P.alloc(1, 128, F32) for _ in range(16)]
            mk2 = P.mark()
            s32 = RR([P.alloc(1, TT, F32) for _ in range(3)])
            s16 = RR([P.alloc(1, TT, BF16) for _ in range(3)])
            L8 = P.alloc(1, 32, F32)
            L8b = P.alloc(1, 32, F32)
            SEL = P.alloc(1, 32, F32)
            M4 = P.alloc(1, 4, F32)
            for t in range(NT):
                lbk = []

                def h32cb(k, tv):
                    if k == 0:
                        for s in range(4):
                            lbk.append(P.acquire())
                    h32 = s32.get().v(0)
                    P.act(h32, tv, AF.Identity, scale=A2[1](k), bias=modc(1, 3, k))
                    for s in range(4):
                        P.mm(P.banks[lbk[s]].v(0, 0, 8), V(h32.ap[:, s * 128:(s + 1) * 128], h32.bufs), RW.v(k),
                             start=(k == 0), stop=(k == 7))
                norm_tile(t * TT, A2[1], lambda k: modc(1, 3, k), lambda k, t=t: H.v(k, t * TT, (t + 1) * TT), s32, s16, h32cb)

                def v3(Tn, n=8):
                    return V(Tn.full[:, 0:4 * n].rearrange("p (s e) -> p s e", s=4), Tn.allbufs())
                rb3 = V(POt.full[:, PO_OFF["rb"]:PO_OFF["rb"] + 8].unsqueeze(1).to_broadcast([128, 4, 8]), POt.allbufs())
                m4b = lambda: V(M4.full.unsqueeze(2).to_broadcast([128, 4, 8]), M4.allbufs())
                for s in range(4):
                    P.tt(L8.v(0, s * 8, s * 8 + 8), P.banks[lbk[s]].v(0, 0, 8), po_col("rb", 0, 8), ALU.add)
                    P.release(lbk[s])
                P.reduce(M4.all(), v3(L8), ALU.max)
                P.tt(v3(SEL), v3(L8), m4b(), ALU.is_equal)
                P.stt(v3(L8b), v3(SEL), -BIG, v3(L8), ALU.mult, ALU.add)
                P.tt(v3(L8), v3(L8), m4b(), ALU.subtract)
                P.act(L8.all(), L8.all(), AF.Exp)
                P.reduce(M4.all(), v3(L8b), ALU.max)
                P.tt(v3(L8b), v3(L8b), m4b(), ALU.is_ge)
                P.tt(v3(SEL), v3(SEL), v3(L8b), ALU.add)
                P.tt(v3(L8), v3(L8), v3(SEL), ALU.mult)
                P.reduce(M4.all(), v3(L8), ALU.add)
                P.recip(M4.all(), M4.all())
                P.tt(V(WTOK.full[:, t * 32:(t + 1) * 32].rearrange("p (s e) -> p s e", s=4), WTOK.allbufs()),
                     v3(L8), m4b(), ALU.mult)
            P.reset(mk2)
            ws = ffn_bufs(True)

            def wb_diag(e):
                for st in range(16):
                    P.ts(D16[st].v(0), ident32, WTOK.v(0, st * 8 + e, st * 8 + e + 1), ALU.mult)

            def wb_mm(e):
                for t in range(NT):
                    bk = P.acquire()
                    for s in range(4):
                        P.mm(P.banks[bk].v(0, s * 128, (s + 1) * 128), ones32, D16[t * 4 + s].v(0))
                    P.act(WBs[e % 2].v(0, t * TT, (t + 1) * TT), P.banks[bk].v(0), AF.Identity)
                    P.release(bk)

            for e in range(8):
                wb_diag(e)
                wb_mm(e)
                ffn_like(H, m_wg[e * 1024:(e + 1) * 1024, :], m_wu[e * 1024:(e + 1) * 1024, :],
                         m_wd[e * 3584:(e + 1) * 3584, :], 28, lambda d: modc(1, 5, d), WBs[e % 2], ws)
            ffn_flush(ws)
            P.reset(mk)

        xr = lambda xd: xd.rearrange("(k p) t -> p k t", p=128)
        for k in range(8):
            P.dma(X.vc(k, k + 1), xr(xA)[:, k:k + 1, :])
        KVTOP = ARENA_BYTES
        if stage >= 1:
            l0_mix(0)
        compute_mod(1)
        if stage >= 2:
            l0_ffn()
        KTA = VVA = KTB = VVB = None
        if stage >= 3:
            KTA = P.alloc(4, T, BF16, tile=256, at=KVTOP - 16384)
            VVA = P.alloc(16, 520, BF16, at=KVTOP - 16384 - 16640)
            P.memset(V(VVA.full.rearrange("p (g e) -> p g e", e=65)[:, :, 64:65], VVA.allbufs()), 1.0)
            l1_kv_A(KTA, VVA)
        for k in range(8):
            P.dma(X.vc(k, k + 1), xr(xB)[:, k:k + 1, :])
        if stage >= 1:
            l0_mix(1)
        if stage >= 2:
            l0_ffn()
        if stage >= 4:
            KTB = P.alloc(4, T, BF16, tile=256, at=KVTOP - 33024 - 16384)
            VVB = P.alloc(16, 520, BF16, at=KVTOP - 33024 - 16384 - 16640)
            P.memset(V(VVB.full.rearrange("p (g e) -> p g e", e=65)[:, :, 64:65], VVB.allbufs()), 1.0)
            l1_mix_B(KTA, VVA, KTB, VVB)
            for tns in (KTA, VVA, KTB, VVB):
                P.free(tns)
        if stage >= 5:
            moe_B()
        outs = []
        for k in range(8):
            outs.append(P.dma(xr(out_d)[:, k:k + 1, :], X.vc(k, k + 1)))
        P.finish(outs)
        P.emit()
    return nc
from concourse.bass_utils import run_bass_kernel_spmd

ROPE_THETA = 10000.0
STAGE = 99
_NC_CACHE = {}


def _cols(v, n):
    return np.ascontiguousarray(np.asarray(v, np.float32).reshape(n, 128).T)


def _const_arrays():
    ident = np.eye(128, dtype=np.float32)
    ones = np.ones((128, 128), np.float32)
    hm = np.zeros((128, 2), np.float32)
    hm[:64, 0] = 1.0
    hm[64:, 1] = 1.0
    c32 = np.concatenate([ident, ones, hm], axis=1)
    blk = np.zeros((128, 128), np.float32)
    blk[:64, :64] = 1.0 / 64
    blk[64:, 64:] = 1.0 / 64
    rperm = np.zeros((128, 128), np.float32)
    for m in range(128):
        p = 64 * (m // 64) + ((m % 64) + 32) % 64
        rperm[p, m] = 1.0
    cb = np.concatenate([ident, ones / 1024.0, ones / 512.0, blk, ones, rperm], axis=1)
    kk = np.arange(128)[:, None]
    uu = np.arange(896)[None, :] - 384
    cc = np.where(kk <= uu, 0.0, -BIG).astype(np.float32)
    ej = np.zeros((128, 16, 128), np.float32)
    for j in range(16):
        ej[j, j, :] = 1.0
    return c32, cb, cc, ej.reshape(128, 2048)


def _rope_tables(sh):
    half = 32
    inv = (np.float32(ROPE_THETA) ** (-np.arange(half, dtype=np.float32) / np.float32(half))).astype(np.float32)
    slots = np.arange(2 * T)
    if sh == 1:
        pos = slots
    else:
        pos = np.where(slots >= T, slots - T, 0)
    ang = pos.astype(np.float32)[None, :] * inv[:, None]
    cos = np.cos(ang).astype(np.float32)
    sin = np.sin(ang).astype(np.float32)
    i = np.arange(128) % 64
    cosT = cos[i % 32]
    sinT = np.where((i < 32)[:, None], -sin[i % 32], sin[i % 32])
    return np.ascontiguousarray(cosT, np.float32), np.ascontiguousarray(sinT, np.float32)


def _prep(inp):
    g = lambda n: np.asarray(inp[n], np.float32)
    x = g("x")
    c = g("c")
    caw = g("e_conv_a_w")[0]
    cbw = g("e_conv_b_w")[0]
    ccw = g("o_conv_c_w")[0]
    pe = np.concatenate([
        _cols(g("e_norm_mix")[0], 8), _cols(g("e_norm_ffn")[0], 8), _cols(g("e_ada_b")[0], 48),
        np.ascontiguousarray(caw.T.reshape(4, 128, 31).transpose(1, 0, 2)).reshape(128, 124),
        _cols(g("e_conv_a_b")[0], 4), _cols(g("e_ln_a_g")[0], 4), _cols(g("e_ln_a_b")[0], 4),
        np.ascontiguousarray(cbw.T.reshape(4, 128, 4).transpose(1, 0, 2)).reshape(128, 16),
        _cols(g("e_conv_b_b")[0], 4), _cols(g("e_lru_ba")[0], 4), _cols(g("e_lru_bx")[0], 4),
        _cols(g("e_lru_lambda")[0], 4)], axis=1)
    assert pe.shape[1] == NPE
    po = np.concatenate([
        _cols(g("o_norm_mix")[0], 8), _cols(g("o_norm_ffn")[0], 8), _cols(g("o_ada_b")[0], 48),
        np.ascontiguousarray(ccw.T.reshape(4, 128, 3).transpose(1, 0, 2)).reshape(128, 12),
        np.tile(g("o_q_norm")[0], 2)[:, None], np.tile(g("o_k_norm")[0], 2)[:, None],
        np.broadcast_to(g("o_router_b")[0][None, :], (128, 8))], axis=1)
    assert po.shape[1] == NPO
    lru = np.zeros((128, 8, 128), np.float32)
    wa = g("e_lru_wa")[0]
    wx = g("e_lru_wx")[0]
    for cch in range(4):
        for hh in range(2):
            lru[hh * 64:(hh + 1) * 64, cch, hh * 64:(hh + 1) * 64] = wa[2 * cch + hh]
            lru[hh * 64:(hh + 1) * 64, 4 + cch, hh * 64:(hh + 1) * 64] = wx[2 * cch + hh]
    c32, cb, cc, ej = _const_arrays()
    shared = {
        "pe": np.ascontiguousarray(pe), "po": np.ascontiguousarray(po),
        "c32": c32, "cb": cb, "cc": cc, "ej": ej,
        "e_ada_w": g("e_ada_w")[0], "o_ada_w": g("o_ada_w")[0],
        "e_w_in": g("e_w_in")[0], "e_w_out": g("e_w_out")[0], "lru_w": lru.reshape(128, 1024),
        "e_ffn_wg": g("e_ffn_wg")[0], "e_ffn_wu": g("e_ffn_wu")[0], "e_ffn_wd": g("e_ffn_wd")[0],
        "o_w_in": g("o_w_in")[0], "o_w_out": g("o_w_out")[0], "o_router_w": g("o_router_w")[0],
    }
    if STAGE >= 5:
        shared["o_moe_wg"] = g("o_moe_wg")[0].reshape(8 * 1024, 3584)
        shared["o_moe_wu"] = g("o_moe_wu")[0].reshape(8 * 1024, 3584)
        shared["o_moe_wd"] = g("o_moe_wd")[0].reshape(8 * 3584, 1024)
    tabs = [_rope_tables(0), _rope_tables(1)]
    maps = []
    for core in range(8):
        b, sh = core // 2, core % 2
        m = dict(shared)
        if sh == 1:
            m["xA"] = np.ascontiguousarray(x[b, :T].T)
        else:
            m["xA"] = np.zeros((1024, T), np.float32)
        m["xB"] = np.ascontiguousarray(x[b, sh * T:(sh + 1) * T].T)
        m["cvec"] = _cols(c[b], 8)
        m["flag"] = np.full((128, 1), float(sh), np.float32)
        bb = np.zeros((128, 16), np.float32)
        if sh == 0:
            bb[:, :8] = -BIG
        m["blkb"] = bb
        m["cos"], m["sin"] = tabs[sh]
        maps.append(m)
    return maps


def kernel(**inputs):
    maps = _prep(inputs)
    if STAGE not in _NC_CACHE:
        _NC_CACHE[STAGE] = build_program(STAGE)
    nc = _NC_CACHE[STAGE]
    res = run_bass_kernel_spmd(nc, maps, core_ids=list(range(8)))
    out = np.empty((4, 2 * T, 1024), np.float32)
    for core in range(8):
        b, sh = core // 2, core % 2
        out[b, sh * T:(sh + 1) * T, :] = np.asarray(res.results[core]["out"], np.float32).T
    return out
```

```python
import numpy as np
from contextlib import ExitStack
import concourse.bass as bass
import concourse.mybir as mybir

F32 = mybir.dt.float32
BF16 = mybir.dt.bfloat16
AF = mybir.ActivationFunctionType
ALU = mybir.AluOpType
AX = mybir.AxisListType

ENGS = ["pe", "act", "dve", "pool", "sp"]
EPOCH = 6000
NDSEM = 14
ARENA_BYTES = 207 * 1024


class Buf:
    __slots__ = ("w", "r")

    def __init__(self):
        self.w = None
        self.r = set()


class V:
    __slots__ = ("ap", "bufs")

    def __init__(self, ap, bufs):
        self.ap = ap
        self.bufs = bufs


class Tens:
    def __init__(self, full_ap, P, C, N, tile):
        self.P, self.C, self.N = P, C, N
        self.tile = tile
        nt = (N + tile - 1) // tile
        self.bufs = [[Buf() for _ in range(nt)] for _ in range(C)]
        self.full = full_ap

    def allbufs(self):
        return [b for bl in self.bufs for b in bl]

    def v(self, c, lo=0, hi=None, p0=0, p1=None):
        if hi is None:
            hi = self.N
        if p1 is None:
            p1 = self.P
        ap = self.full[p0:p1, c * self.N + lo: c * self.N + hi]
        bl = self.bufs[c][lo // self.tile: (hi - 1) // self.tile + 1]
        return V(ap, list(bl))

    def vc(self, c0, c1, lo=0, hi=None, p0=0, p1=None):
        if hi is None:
            hi = self.N
        if p1 is None:
            p1 = self.P
        ap = self.full[p0:p1, c0 * self.N: c1 * self.N].rearrange("p (c n) -> p c n", c=c1 - c0)[:, :, lo:hi]
        bl = []
        for c in range(c0, c1):
            bl += self.bufs[c][lo // self.tile: (hi - 1) // self.tile + 1]
        return V(ap, bl)

    def all(self):
        return V(self.full, self.allbufs())


def _compress(deps, ops):
    best = {}
    out = set()
    for (e, i) in deps:
        if ops[e][i]["dma"]:
            out.add((e, i))
        else:
            if e not in best or best[e] < i:
                best[e] = i
    for e, i in best.items():
        out.add((e, i))
    return out


class Prog:
    def __init__(self, nc, es):
        self.nc = nc
        self.es = es
        self.ops = {e: [] for e in ENGS}
        self.ndma = {"sp": 0, "pool": 0}
        self.arena = es.enter_context(nc.sbuf_tensor("arena", [128, ARENA_BYTES // 2], BF16))
        self.arena_ap = self.arena[:]
        self.top = 0
        self.live = []
        self.retired = []
        self.banks = []
        for i in range(8):
            h = es.enter_context(nc.psum_tensor(f"bank{i}", [128, 512], F32))
            self.banks.append(Tens(h[:], 128, 1, 512, 512))
        self.free_banks = list(range(8))

    def acquire(self):
        assert self.free_banks, "out of PSUM banks"
        i = self.free_banks.pop(0)
        return i

    def release(self, i):
        self.free_banks.append(i)

    def alloc(self, C, N, dtype, tile=None, P=128, at=None):
        esz = 4 if dtype == F32 else 2
        nbytes = C * N * esz
        nbytes_al = (nbytes + 31) // 32 * 32
        if at is None:
            lo = self.top
            self.top += nbytes_al
        else:
            lo = at
        hi = lo + nbytes_al
        assert hi <= ARENA_BYTES, f"arena overflow {hi}"
        for (l2, h2, _) in self.live:
            assert hi <= l2 or lo >= h2, f"overlap with live tensor [{l2},{h2}) vs [{lo},{hi})"
        ap = self.arena_ap[0:P, lo // 2: (lo + nbytes) // 2]
        if dtype == F32:
            ap = ap.bitcast(F32)
        t = Tens(ap, P, C, N, tile or N)
        seeds = set()
        keep = []
        for (l2, h2, t2) in self.retired:
            if hi <= l2 or lo >= h2:
                keep.append((l2, h2, t2))
                continue
            for b in t2.allbufs():
                if b.w is not None:
                    seeds.add(b.w)
                seeds |= b.r
            if not (lo <= l2 and h2 <= hi):
                keep.append((l2, h2, t2))
        self.retired = keep
        if seeds:
            seeds = _compress(seeds, self.ops)
            for b in t.allbufs():
                b.r = set(seeds)
        self.live.append((lo, hi, t))
        return t

    def mark(self):
        return (self.top, len(self.live))

    def reset(self, mark):
        top, n = mark
        for ent in self.live[n:]:
            self.retired.append(ent)
        self.live = self.live[:n]
        self.top = top

    def free(self, t):
        for k, ent in enumerate(self.live):
            if ent[2] is t:
                self.retired.append(ent)
                del self.live[k]
                return
        raise KeyError

    def op(self, eng, fn, reads=(), writes=(), dma=False):
        idx = len(self.ops[eng])
        deps = set()
        for v in reads:
            for b in v.bufs:
                if b.w is not None:
                    deps.add(b.w)
        for v in writes:
            for b in v.bufs:
                if b.w is not None:
                    deps.add(b.w)
                deps |= b.r
        me = (eng, idx)
        deps.discard(me)
        if eng == "pe":
            deps = {d for d in deps if d[0] != "pe"}
        deps = _compress(deps, self.ops)
        o = {"fn": fn, "deps": deps, "sig": False, "dma": dma}
        if dma:
            q = self.ndma[eng]
            self.ndma[eng] += 1
            o["dq"] = q
        self.ops[eng].append(o)
        for v in reads:
            for b in v.bufs:
                if not dma:
                    b.r = {x for x in b.r if x[0] != eng or self.ops[eng][x[1]]["dma"]}
                b.r.add(me)
        for v in writes:
            for b in v.bufs:
                b.w = me
                b.r = set()
        return me

    def emit(self):
        nc = self.nc
        ops = self.ops
        for e in ENGS:
            for o in ops[e]:
                for (de, di) in o["deps"]:
                    ops[de][di]["sig"] = True
        nsig = {}
        for e in ENGS:
            r = 0
            for i, o in enumerate(ops[e]):
                if o["sig"] and not o["dma"]:
                    r += 1
                    o["rank"] = r
            nsig[e] = r
        es = self.es
        esem = {}
        for e in ENGS:
            n_ep = (nsig[e] + EPOCH - 1) // EPOCH
            esem[e] = [es.enter_context(nc.semaphore(f"s_{e}_{k}")) for k in range(max(n_ep, 1))]
        dsem = {q: [es.enter_context(nc.semaphore(f"d_{q}_{k}")) for k in range(NDSEM)] for q in ("sp", "pool")}
        block = es.enter_context(nc.Block())
        self.stats = {e: len(ops[e]) for e in ENGS}
        self.stats["nsig"] = nsig

        def run(eng, e):
            waited_rank = {x: 0 for x in ENGS}
            waited_dma = {}
            for o in ops[eng]:
                for (de, di) in sorted(o["deps"]):
                    d = ops[de][di]
                    if d["dma"]:
                        q = d["dq"]
                        k = q % NDSEM
                        val = 16 * (q // NDSEM + 1)
                        if waited_dma.get((de, k), 0) < val:
                            e.wait_ge(dsem[de][k], val)
                            waited_dma[(de, k)] = val
                    else:
                        r = d["rank"]
                        if waited_rank[de] < r:
                            ep = (r - 1) // EPOCH
                            e.wait_ge(esem[de][ep], r - ep * EPOCH)
                            waited_rank[de] = r
                if o["dma"]:
                    q = o["dq"]
                    k = q % NDSEM
                    prev = 16 * (q // NDSEM)
                    if prev > 0 and waited_dma.get((eng, k), 0) < prev:
                        e.wait_ge(dsem[eng][k], prev)
                        waited_dma[(eng, k)] = prev
                    ins = o["fn"](e)
                    ins.then_inc(dsem[eng][k], 16)
                elif o["fn"] is not None:
                    ins = o["fn"](e)
                    if o["sig"]:
                        r = o["rank"]
                        ep = (r - 1) // EPOCH
                        ins.then_inc(esem[eng][ep], 1)

        @block.tensor
        def _(e):
            run("pe", e)

        @block.scalar
        def _(e):
            run("act", e)

        @block.vector
        def _(e):
            run("dve", e)

        @block.gpsimd
        def _(e):
            run("pool", e)

        @block.sync
        def _(e):
            run("sp", e)

    def mm(self, out, lhsT, rhs, start=True, stop=True):
        return self.op("pe", lambda e: e.matmul(out.ap, lhsT.ap, rhs.ap, start=start, stop=stop),
                       reads=[lhsT, rhs], writes=[out])

    def transpose(self, out, in_, ident):
        return self.op("pe", lambda e: e.transpose(out.ap, in_.ap, ident.ap), reads=[in_, ident], writes=[out])

    def act(self, out, in_, func, scale=1.0, bias=0.0):
        reads = [in_]
        sc = scale
        bi = bias
        if isinstance(scale, V):
            reads.append(scale)
            sc = scale.ap
        if isinstance(bias, V):
            reads.append(bias)
            bi = bias.ap
        return self.op("act", lambda e: e.activation(out.ap, in_.ap, func, bias=bi, scale=sc),
                       reads=reads, writes=[out])

    def tt(self, out, a, b, op, eng="dve"):
        return self.op(eng, lambda e: e.tensor_tensor(out.ap, a.ap, b.ap, op), reads=[a, b], writes=[out])

    def ts(self, out, a, s1, op0, s2=None, op1=None, eng="dve"):
        reads = [a]
        x1 = s1
        x2 = s2
        if isinstance(s1, V):
            reads.append(s1)
            x1 = s1.ap
        if isinstance(s2, V):
            reads.append(s2)
            x2 = s2.ap
        if op1 is None:
            return self.op(eng, lambda e: e.tensor_scalar(out.ap, a.ap, x1, None, op0), reads=reads, writes=[out])
        return self.op(eng, lambda e: e.tensor_scalar(out.ap, a.ap, x1, x2, op0, op1), reads=reads, writes=[out])

    def stt(self, out, a, s, b, op0, op1):
        reads = [a, b]
        x = s
        if isinstance(s, V):
            reads.append(s)
            x = s.ap
        return self.op("dve", lambda e: e.scalar_tensor_tensor(out.ap, a.ap, x, b.ap, op0, op1), reads=reads, writes=[out])

    def copy(self, out, a, eng="dve"):
        if eng == "act":
            return self.act(out, a, AF.Identity)
        return self.op(eng, lambda e: e.tensor_copy(out.ap, a.ap), reads=[a], writes=[out])

    def memset(self, out, val, eng="dve"):
        return self.op(eng, lambda e: e.memset(out.ap, val), reads=[], writes=[out])

    def reduce(self, out, a, op, eng="dve"):
        return self.op(eng, lambda e: e.tensor_reduce(out.ap, a.ap, AX.X, op), reads=[a], writes=[out])

    def recip(self, out, a):
        return self.op("dve", lambda e: e.reciprocal(out.ap, a.ap), reads=[a], writes=[out])

    def scan(self, out, d0, d1, init):
        reads = [d0, d1]
        x = init
        if isinstance(init, V):
            reads.append(init)
            x = init.ap
        return self.op("dve", lambda e: e.tensor_tensor_scan(out.ap, d0.ap, d1.ap, x, ALU.mult, ALU.add),
                       reads=reads, writes=[out])

    def dma(self, out, in_, q="sp"):
        reads = [in_] if isinstance(in_, V) else []
        writes = [out] if isinstance(out, V) else []
        oa = out.ap if isinstance(out, V) else out
        ia = in_.ap if isinstance(in_, V) else in_
        return self.op(q, lambda e: e.dma_start(out=oa, in_=ia), reads=reads, writes=writes, dma=True)

    def finish(self, out_dma_ops):
        deps = set(out_dma_ops)
        self.ops["sp"].append({"fn": None, "deps": deps, "sig": False, "dma": False})

T = 2048
TT = 512
NT = T // TT
BIG = 30000.0
EPS = 1e-6

PE_OFF = {}
_o = 0
for _n, _w in [("nm", 8), ("nf", 8), ("adab", 48), ("caw", 124), ("cab", 4), ("lng", 4), ("lnb", 4),
               ("cbw", 16), ("cbb", 4), ("lba", 4), ("lbx", 4), ("lam", 4)]:
    PE_OFF[_n] = _o
    _o += _w
NPE = _o
PO_OFF = {}
_o = 0
for _n, _w in [("nm", 8), ("nf", 8), ("adab", 48), ("ccw", 12), ("qn", 1), ("kn", 1), ("rb", 8)]:
    PO_OFF[_n] = _o
    _o += _w
NPO = _o


class RR:
    def __init__(self, tiles):
        self.t = tiles
        self.i = 0

    def get(self):
        x = self.t[self.i % len(self.t)]
        self.i += 1
        return x


def build_program(stage=99):
    nc = bass.Bass("TRN2", target_bir_lowering=False)

    def din(name, shape):
        return nc.dram_tensor(name, shape, F32, kind="ExternalInput").ap()

    xA = din("xA", [1024, T])
    xB = din("xB", [1024, T])
    cvec = din("cvec", [128, 8])
    flag_d = din("flag", [128, 1])
    pe_d = din("pe", [128, NPE])
    po_d = din("po", [128, NPO])
    c32_d = din("c32", [128, 258])
    cb_d = din("cb", [128, 6 * 128])
    cc_d = din("cc", [128, 896])
    ej_d = din("ej", [128, 2048])
    blkb_d = din("blkb", [128, 16])
    cos_d = din("cos", [128, 2 * T])
    sin_d = din("sin", [128, 2 * T])
    e_ada_w = din("e_ada_w", [1024, 6144])
    o_ada_w = din("o_ada_w", [1024, 6144])
    e_w_in = din("e_w_in", [1024, 2048])
    e_w_out = din("e_w_out", [1024, 1024])
    lru_w = din("lru_w", [128, 8 * 128])
    e_wg = din("e_ffn_wg", [1024, 2816])
    e_wu = din("e_ffn_wu", [1024, 2816])
    e_wd = din("e_ffn_wd", [2816, 1024])
    o_w_in = din("o_w_in", [1024, 3072])
    o_w_out = din("o_w_out", [1024, 1024])
    o_rw = din("o_router_w", [1024, 8])
    if stage >= 5:
        m_wg = din("o_moe_wg", [8 * 1024, 3584])
        m_wu = din("o_moe_wu", [8 * 1024, 3584])
        m_wd = din("o_moe_wd", [8 * 3584, 1024])
    out_d = nc.dram_tensor("out", [1024, T], F32, kind="ExternalOutput").ap()

    es = ExitStack()
    with es:
        P = Prog(nc, es)
        X = P.alloc(8, T, F32, tile=TT)
        CB = P.alloc(6, 128, BF16)
        CC = P.alloc(1, 896, BF16)
        EJ = P.alloc(16, 128, BF16)
        C32 = P.alloc(1, 258, F32)
        PEt = P.alloc(1, NPE, F32)
        POt = P.alloc(1, NPO, F32)
        FLG = P.alloc(1, 1, F32)
        BLKB = P.alloc(1, 16, F32)
        CACT = P.alloc(1, 8, BF16)
        MOD = [P.alloc(1, 48, F32), P.alloc(1, 48, F32)]
        DER = P.alloc(1, 64, F32)
        LST = P.alloc(4, 1, F32)
        YAH = P.alloc(4, 30, BF16)
        BXH = P.alloc(4, 3, BF16)
        ZH = P.alloc(4, 2, BF16)
        KMT = P.alloc(4, 16, F32)
        KMTB = P.alloc(4, 16, BF16)
        KMH = [P.alloc(4, 16, BF16), P.alloc(4, 16, BF16)]
        WTOK = P.alloc(1, 128, F32)
        LRUW = P.alloc(8, 128, BF16)
        RW = P.alloc(8, 8, F32)

        identb, ones1024, ones512, blk64, onesb, rperm = [CB.v(i) for i in range(6)]
        ident32, ones32 = C32.v(0, 0, 128), C32.v(0, 128, 256)
        hmask = [C32.v(0, 256, 257), C32.v(0, 257, 258)]

        def pe_col(name, j=0, n=1):
            o = PE_OFF[name] + j
            return PEt.v(0, o, o + n)

        def po_col(name, j=0, n=1):
            o = PO_OFF[name] + j
            return POt.v(0, o, o + n)

        def der(j, n=1):
            return DER.v(0, j, j + n)

        P.dma(CB.all(), cb_d, q="pool")
        P.dma(CC.all(), cc_d, q="pool")
        P.dma(EJ.all(), ej_d, q="pool")
        P.dma(LRUW.all(), lru_w, q="pool")
        P.dma(C32.all(), c32_d)
        P.dma(PEt.all(), pe_d)
        P.dma(POt.all(), po_d)
        P.dma(FLG.all(), flag_d)
        P.dma(BLKB.all(), blkb_d)
        P.dma(RW.vc(0, 8), o_rw.rearrange("(k p) e -> p k e", p=128))
        P.memset(KMTB.all(), 0.0)
        P.memset(KMH[0].all(), 0.0)
        P.memset(KMH[1].all(), 0.0)

        m0 = P.mark()
        CV = P.alloc(1, 8, F32)
        P.dma(CV.all(), cvec)
        P.act(CACT.all(), CV.all(), AF.Silu)
        def compute_mod(L):
            mkm = P.mark()
            WS = [P.alloc(8, 1024, BF16), P.alloc(8, 1024, BF16)]
            wi = 0
            adaw = [e_ada_w, o_ada_w][L]
            bk = P.acquire()
            bank = P.banks[bk]
            adaw_r = adaw.rearrange("(k p) n -> p k n", p=128)
            for v6 in range(6):
                w = WS[wi % 2]
                wi += 1
                P.dma(w.vc(0, 8), adaw_r[:, :, v6 * 1024:(v6 + 1) * 1024], q="pool")
                for mc in range(8):
                    col = v6 * 8 + mc
                    for k in range(8):
                        P.mm(bank.v(0, col, col + 1), w.v(k, mc * 128, (mc + 1) * 128), CACT.v(0, k, k + 1),
                             start=(k == 0), stop=(k == 7))
            par = PEt if L == 0 else POt
            off = (PE_OFF if L == 0 else PO_OFF)["adab"]
            P.tt(MOD[L].all(), bank.v(0, 0, 48), par.v(0, off, off + 48), ALU.add)
            P.release(bk)
            offm = (PE_OFF if L == 0 else PO_OFF)
            P.stt(der(L * 16, 8), MOD[L].v(0, 8, 16), 1.0, par.v(0, offm["nm"], offm["nm"] + 8), ALU.add, ALU.mult)
            P.stt(der(L * 16 + 8, 8), MOD[L].v(0, 32, 40), 1.0, par.v(0, offm["nf"], offm["nf"] + 8), ALU.add, ALU.mult)
            P.reset(mkm)

        compute_mod(0)
        lam = pe_col("lam", 0, 4)
        t_abs = der(40, 4)
        t_e = der(44, 4)
        t_mx = der(48, 4)
        P.ts(t_mx, lam, -1.0, ALU.mult)
        P.tt(t_abs, lam, t_mx, ALU.max)
        P.act(t_e, t_abs, AF.Exp, scale=-1.0)
        P.act(t_e, t_e, AF.Ln, bias=1.0)
        P.ts(t_mx, lam, -1.0, ALU.mult, 0.0, ALU.max)
        P.tt(t_e, t_e, t_mx, ALU.add)
        P.ts(der(32, 4), t_e, -8.0, ALU.mult)
        P.ts(der(36, 4), t_e, -16.0, ALU.mult)
        P.reset(m0)

        A1 = [lambda k, L=L: der(L * 16 + k) for L in range(2)]
        A2 = [lambda k, L=L: der(L * 16 + 8 + k) for L in range(2)]

        def modc(L, which, k):
            return MOD[L].v(0, which * 8 + k, which * 8 + k + 1)

        def rstd_from_bank(bank, outR):
            P.act(outR, bank, AF.Ln, bias=EPS)
            P.act(outR, outR, AF.Exp, scale=-0.5)

        RN = P.alloc(1, TT, F32)

        def norm_tile(lo, Acol, shcol, hout, s32, s16, h32cb=None):
            bk = P.acquire()
            bank = P.banks[bk].v(0)
            for k in range(8):
                sq = s16.get().v(0)
                P.act(sq, X.v(k, lo, lo + TT), AF.Square)
                P.mm(bank, ones1024, sq, start=(k == 0), stop=(k == 7))
            R = RN.v(0)
            rstd_from_bank(bank, R)
            P.release(bk)
            for k in range(8):
                t = s32.get().v(0)
                P.tt(t, X.v(k, lo, lo + TT), R, ALU.mult)
                P.act(hout(k), t, AF.Identity, scale=Acol(k), bias=shcol(k))
                if h32cb is not None:
                    h32cb(k, t)

        def stream_cols(w_d, c0, c1, dst):
            P.dma(dst.vc(0, 8, 0, c1 - c0), w_d.rearrange("(k p) n -> p k n", p=128)[:, :, c0:c1], q="pool")

        def proj_group(h, w, nchunk, N=TT):
            bks = []
            for c in range(nchunk):
                bk = P.acquire()
                for k in range(8):
                    P.mm(P.banks[bk].v(0, 0, N), w.v(k, c * 128, (c + 1) * 128), h(k), start=(k == 0), stop=(k == 7))
                bks.append(bk)
            return bks

        def out_proj(w_d, mix, gcol, lo, wbuf):
            wbuf_in = wbuf
            for half in range(2):
                wbuf = wbuf_in() if callable(wbuf_in) else wbuf_in
                stream_cols(w_d, half * 512, (half + 1) * 512, wbuf)
                for dc in range(4):
                    d = half * 4 + dc
                    bk = P.acquire()
                    for k in range(8):
                        P.mm(P.banks[bk].v(0), wbuf.v(k, dc * 128, (dc + 1) * 128), mix.v(k), start=(k == 0), stop=(k == 7))
                    P.stt(X.v(d, lo, lo + TT), P.banks[bk].v(0), gcol(d), X.v(d, lo, lo + TT), ALU.mult, ALU.add)
                    P.release(bk)

        def ffn_like(H, wg_d, wu_d, wd_d, F, gcol, WB, ws, nt=NT, ghook=None):
            ngr = (F + 3) // 4
            for g in range(ngr):
                gc = min(4, F - g * 4)
                slot = ws["i"] % 2
                ws["i"] += 1
                wgb, wub, wdb = ws["g"][slot], ws["u"][slot], ws["d"][slot]
                c0 = g * 512
                c1 = c0 + gc * 128
                P.dma(wgb.vc(0, 8, 0, gc * 128), wg_d.rearrange("(k p) n -> p k n", p=128)[:, :, c0:c1], q="pool")
                P.dma(wub.vc(0, 8, 0, gc * 128), wu_d.rearrange("(k p) n -> p k n", p=128)[:, :, c0:c1], q="pool")
                P.dma(wdb.vc(0, gc), wd_d[c0:c1, :].rearrange("(f p) d -> p f d", p=128), q="pool")
                if ghook is not None:
                    ghook(g)
                for tt in range(nt):
                    lo = tt * TT
                    actb = ws["act"].get()
                    for fc in range(gc):
                        bg = P.acquire()
                        bu = P.acquire()
                        for k in range(8):
                            P.mm(P.banks[bg].v(0), wgb.v(k, fc * 128, (fc + 1) * 128), H.v(k, lo, lo + TT), start=(k == 0), stop=(k == 7))
                        for k in range(8):
                            P.mm(P.banks[bu].v(0), wub.v(k, fc * 128, (fc + 1) * 128), H.v(k, lo, lo + TT), start=(k == 0), stop=(k == 7))
                        s = ws["s16"].get().v(0)
                        P.act(s, P.banks[bg].v(0), AF.Silu)
                        P.release(bg)
                        if WB is None:
                            P.tt(actb.v(fc), P.banks[bu].v(0), s, ALU.mult)
                        else:
                            u2 = ws["s32"].get().v(0)
                            P.tt(u2, P.banks[bu].v(0), WB.v(0, lo, lo + TT), ALU.mult)
                            P.tt(actb.v(fc), u2, s, ALU.mult)
                        P.release(bu)
                    ffn_flush(ws)
                    ws["pend"] = (wdb, actb, gc, lo, gcol)

        def ffn_flush(ws):
            if ws.get("pend") is None:
                return
            wdb, actb, gc, lo, gcol = ws["pend"]
            ws["pend"] = None
            for d in range(8):
                bk = P.acquire()
                for fc in range(gc):
                    P.mm(P.banks[bk].v(0), wdb.v(fc, d * 128, (d + 1) * 128), actb.v(fc), start=(fc == 0), stop=(fc == gc - 1))
                P.stt(X.v(d, lo, lo + TT), P.banks[bk].v(0), gcol(d), X.v(d, lo, lo + TT), ALU.mult, ALU.add)
                P.release(bk)

        def ffn_bufs(wb=False):
            return {"i": 0,
                    "g": [P.alloc(8, 512, BF16), P.alloc(8, 512, BF16)],
                    "u": [P.alloc(8, 512, BF16), P.alloc(8, 512, BF16)],
                    "d": [P.alloc(4, 1024, BF16), P.alloc(4, 1024, BF16)],
                    "act": RR([P.alloc(4, 512, BF16), P.alloc(4, 512, BF16)]),
                    "pend": None,
                    "s16": RR([P.alloc(1, 512, BF16) for _ in range(3)]),
                    "s32": RR([P.alloc(1, 512, F32) for _ in range(2)]) if wb else None}

        def l0_mix(seg):
            mk = P.mark()
            s32 = RR([P.alloc(1, TT, F32) for _ in range(10)])
            s16 = RR([P.alloc(1, TT, BF16) for _ in range(6)])
            Ht = P.alloc(8, TT, BF16)
            MIX = P.alloc(8, TT, BF16)
            YA = P.alloc(4, 30 + TT, BF16)
            BX = P.alloc(4, 3 + TT, BF16)
            WI = [P.alloc(8, 512, BF16), P.alloc(8, 512, BF16), P.alloc(8, 512, BF16)]
            ACC = P.alloc(4, TT, F32)
            XB32 = P.alloc(4, TT, F32)
            wi = 0
            if seg == 0:
                for c in range(4):
                    P.memset(YAH.v(c), 0.0)
                    P.memset(BXH.v(c), 0.0)
                    P.memset(LST.v(c), 0.0)
            else:
                for c in range(4):
                    P.ts(YAH.v(c), YAH.v(c), FLG.v(0), ALU.mult)
                    P.ts(BXH.v(c), BXH.v(c), FLG.v(0), ALU.mult)
                    P.ts(LST.v(c), LST.v(c), FLG.v(0), ALU.mult)
            for t in range(NT):
                lo = t * TT
                norm_tile(lo, A1[0], lambda k: modc(0, 0, k), lambda k: Ht.v(k), s32, s16)
                hk = lambda k: Ht.v(k)
                for c in range(4):
                    P.copy(YA.v(c, 0, 30), YAH.v(c), eng="act")
                    P.copy(BX.v(c, 0, 3), BXH.v(c), eng="act")
                w = WI[wi % 3]; wi += 1
                stream_cols(e_w_in, 512, 1024, w)
                bks = proj_group(hk, w, 4)
                sg = []
                for c in range(4):
                    s = s16.get().v(0)
                    P.act(s, P.banks[bks[c]].v(0), AF.Sigmoid)
                    P.release(bks[c])
                    sg.append(s)
                w = WI[wi % 3]; wi += 1
                stream_cols(e_w_in, 0, 512, w)
                bks = proj_group(hk, w, 4)
                for c in range(4):
                    P.tt(YA.v(c, 30, 30 + TT), P.banks[bks[c]].v(0), sg[c], ALU.mult)
                    P.release(bks[c])
                    P.copy(YAH.v(c), YA.v(c, TT, TT + 30), eng="act")
                w = WI[wi % 3]; wi += 1
                stream_cols(e_w_in, 1024, 1536, w)
                bks = proj_group(hk, w, 4)
                for c in range(4):
                    P.act(BX.v(c, 3, 3 + TT), P.banks[bks[c]].v(0), AF.Identity)
                    P.release(bks[c])
                    P.copy(BXH.v(c), BX.v(c, TT, TT + 3), eng="act")
                NA = 15
                cbk = [P.acquire() for _ in range(4)]
                for j in range(31):
                    for c in range(4):
                        wcol = pe_col("caw", c * 31 + j)
                        if j < NA:
                            tmpj = s16.get().v(0)
                            P.act(tmpj, YA.v(c, j, j + TT), AF.Identity, scale=wcol)
                            P.mm(P.banks[cbk[c]].v(0), identb, tmpj, start=(j == 0), stop=(j == NA - 1))
                        elif j == NA:
                            P.ts(ACC.v(c), YA.v(c, j, j + TT), wcol, ALU.mult, pe_col("cab", c), ALU.add)
                        else:
                            P.stt(ACC.v(c), YA.v(c, j, j + TT), wcol, ACC.v(c), ALU.mult, ALU.add)
                for c in range(4):
                    P.tt(ACC.v(c), ACC.v(c), P.banks[cbk[c]].v(0), ALU.add)
                    P.release(cbk[c])
                for j in range(4):
                    for c in range(4):
                        wcol = pe_col("cbw", c * 4 + j)
                        if j == 0:
                            P.ts(XB32.v(c), BX.v(c, 0, TT), wcol, ALU.mult, pe_col("cbb", c), ALU.add)
                        else:
                            P.stt(XB32.v(c), BX.v(c, j, j + TT), wcol, XB32.v(c), ALU.mult, ALU.add)
                bm = P.acquire()
                bq = P.acquire()
                for c in range(4):
                    yb = s16.get().v(0)
                    ysq = s16.get().v(0)
                    P.act(yb, ACC.v(c), AF.Identity)
                    P.act(ysq, ACC.v(c), AF.Square)
                    P.mm(P.banks[bm].v(0), ones512, yb, start=(c == 0), stop=(c == 3))
                    P.mm(P.banks[bq].v(0), ones512, ysq, start=(c == 0), stop=(c == 3))
                Msb = s32.get().v(0)
                P.act(Msb, P.banks[bm].v(0), AF.Identity)
                P.release(bm)
                m2 = s32.get().v(0)
                P.tt(m2, Msb, Msb, ALU.mult)
                var = s32.get().v(0)
                P.tt(var, P.banks[bq].v(0), m2, ALU.subtract)
                P.release(bq)
                P.ts(var, var, 0.0, ALU.max)
                rstd_from_bank(var, var)
                for c in range(4):
                    tq = s32.get().v(0)
                    P.tt(tq, ACC.v(c), Msb, ALU.subtract)
                    P.tt(tq, tq, var, ALU.mult)
                    P.act(MIX.v(c), tq, AF.Silu, scale=pe_col("lng", c), bias=pe_col("lnb", c))
                w = WI[wi % 3]; wi += 1
                stream_cols(e_w_in, 1536, 2048, w)
                bgs = proj_group(hk, w, 4)
                for c in range(4):
                    xbb = s16.get().v(0)
                    P.act(xbb, XB32.v(c), AF.Identity)
                    br = P.acquire()
                    bi = P.acquire()
                    P.mm(P.banks[br].v(0), LRUW.v(c), xbb)
                    P.mm(P.banks[bi].v(0), LRUW.v(4 + c), xbb)
                    r = s32.get().v(0)
                    ii = s32.get().v(0)
                    P.act(r, P.banks[br].v(0), AF.Sigmoid, bias=pe_col("lba", c))
                    P.act(ii, P.banks[bi].v(0), AF.Sigmoid, bias=pe_col("lbx", c))
                    P.release(br)
                    P.release(bi)
                    a = s32.get().v(0)
                    a2 = s32.get().v(0)
                    P.act(a, r, AF.Exp, scale=der(32 + c))
                    P.act(a2, r, AF.Exp, scale=der(36 + c))
                    P.ts(a2, a2, -1.0, ALU.mult, 1.0, ALU.add)
                    P.ts(a2, a2, 0.0, ALU.max)
                    P.act(a2, a2, AF.Sqrt)
                    P.tt(ii, ii, XB32.v(c), ALU.mult)
                    P.tt(ii, ii, a2, ALU.mult)
                    hl = s32.get().v(0)
                    P.scan(hl, a, ii, LST.v(c))
                    P.copy(LST.v(c), V(hl.ap[:, TT - 1:TT], hl.bufs))
                    gl = s32.get().v(0)
                    P.act(gl, P.banks[bgs[c]].v(0), AF.Gelu_apprx_tanh)
                    P.release(bgs[c])
                    P.tt(MIX.v(4 + c), gl, hl, ALU.mult)
                def nextw0():
                    nonlocal wi
                    w_ = WI[wi % 3]
                    wi += 1
                    return w_
                out_proj(e_w_out, MIX, lambda d: modc(0, 2, d), lo, nextw0)
            P.reset(mk)

        def l0_ffn():
            mk = P.mark()
            H = P.alloc(8, T, BF16, tile=TT)
            mk2 = P.mark()
            s32 = RR([P.alloc(1, TT, F32) for _ in range(3)])
            s16 = RR([P.alloc(1, TT, BF16) for _ in range(3)])
            for t in range(NT):
                norm_tile(t * TT, A2[0], lambda k: modc(0, 3, k), lambda k, t=t: H.v(k, t * TT, (t + 1) * TT), s32, s16)
            P.reset(mk2)
            ws = ffn_bufs()
            ffn_like(H, e_wg, e_wu, e_wd, 22, lambda d: modc(0, 5, d), None, ws)
            ffn_flush(ws)
            P.reset(mk)

        def qk_finish(bks, dst, wn_col, slot0, s32, s16, cosT, sinT):
            for c in range(4):
                raw = s32.get().v(0)
                P.act(raw, P.banks[bks[c]].v(0), AF.Identity)
                P.release(bks[c])
                sq = s16.get().v(0)
                P.act(sq, raw, AF.Square)
                qw = s16.get().v(0)
                P.ts(qw, raw, wn_col, ALU.mult)
                bms = P.acquire()
                brt = P.acquire()
                P.mm(P.banks[bms].v(0), blk64, sq)
                P.mm(P.banks[brt].v(0), rperm, qw)
                R = s32.get().v(0)
                rstd_from_bank(P.banks[bms].v(0), R)
                P.release(bms)
                t1 = s32.get().v(0)
                P.tt(t1, qw, cosT, ALU.mult)
                t2 = raw
                P.tt(t2, P.banks[brt].v(0), sinT, ALU.mult)
                P.release(brt)
                P.tt(t1, t1, t2, ALU.add)
                P.tt(dst(c), t1, R, ALU.mult)

        def kv_tile(hk, slot_tile, KTs, VVs, WI, wstate, s32, s16, cosT, sinT, part="kv"):
            lo = slot_tile * TT
            if "k" in part:
                w = WI[wstate[0] % len(WI)]; wstate[0] += 1
                stream_cols(o_w_in, 2048, 2560, w)
                bks = proj_group(hk, w, 4)
                qk_finish(bks, lambda c: KTs.v(c, lo, lo + TT), po_col("kn"), None, s32, s16, cosT, sinT)
            if "v" not in part:
                return
            w = WI[wstate[0] % len(WI)]; wstate[0] += 1
            stream_cols(o_w_in, 2560, 3072, w)
            for s in range(4):
                bk = P.acquire()
                for k in range(8):
                    hv = hk(k)
                    P.mm(P.banks[bk].v(0), V(hv.ap[:, s * 128:(s + 1) * 128], hv.bufs), w.v(k), start=(k == 0), stop=(k == 7))
                ch_ = slot_tile * 4 + s
                vdst = V(VVs.full[:, ch_ * 520:(ch_ + 1) * 520].rearrange("p (h e) -> p h e", h=8)[:, :, 0:64], VVs.v(ch_).bufs)
                vsrc = V(P.banks[bk].full[:, 0:512].rearrange("p (h e) -> p h e", h=8), P.banks[bk].allbufs())
                P.act(vdst, vsrc, AF.Identity)
                P.release(bk)

        def kmean_tile(KTs, seg, slot_tile):
            for c in range(4):
                for hb in range(2):
                    blk = seg * 8 + slot_tile * 2 + hb
                    lo = slot_tile * TT + hb * 256
                    P.reduce(KMT.v(c, blk, blk + 1), KTs.v(c, lo, lo + 256), ALU.add)
                    P.act(KMTB.v(c, blk, blk + 1), KMT.v(c, blk, blk + 1), AF.Identity, scale=1.0 / 256.0)
                    for par in range(2):
                        P.ts(KMH[par].v(c, blk, blk + 1), KMT.v(c, blk, blk + 1), hmask[par], ALU.mult, 1.0 / 256.0, ALU.mult)

        def load_cs(seg, t, COS, SIN):
            s0 = seg * T + t * TT
            P.dma(COS.all(), cos_d[:, s0:s0 + TT])
            P.dma(SIN.all(), sin_d[:, s0:s0 + TT])

        def l1_kv_A(KTA, VVA):
            mk = P.mark()
            s32 = RR([P.alloc(1, TT, F32) for _ in range(6)])
            s16 = RR([P.alloc(1, TT, BF16) for _ in range(4)])
            Ht = P.alloc(8, TT, BF16)
            WI = [P.alloc(8, 512, BF16), P.alloc(8, 512, BF16)]
            COS = P.alloc(1, TT, F32)
            SIN = P.alloc(1, TT, F32)
            wst = [0]
            for t in range(NT):
                load_cs(0, t, COS, SIN)
                norm_tile(t * TT, A1[1], lambda k: modc(1, 0, k), lambda k: Ht.v(k), s32, s16)
                kv_tile(lambda k: Ht.v(k), t, KTA, VVA, WI, wst, s32, s16, COS.v(0), SIN.v(0))
                kmean_tile(KTA, 0, t)
                if t == NT - 1:
                    hk = lambda k: V(Ht.v(k).ap[:, TT - 128:TT], Ht.v(k).bufs)
                    w = WI[wst[0] % 2]; wst[0] += 1
                    stream_cols(o_w_in, 0, 512, w)
                    bks = proj_group(hk, w, 4, N=128)
                    chs = []
                    for c in range(4):
                        sx = s16.get().v(0)
                        P.act(V(sx.ap[:, 0:128], sx.bufs), P.banks[bks[c]].v(0, 0, 128), AF.Identity)
                        P.release(bks[c])
                        chs.append(sx)
                    w = WI[wst[0] % 2]; wst[0] += 1
                    stream_cols(o_w_in, 1024, 1536, w)
                    bks = proj_group(hk, w, 4, N=128)
                    for c in range(4):
                        P.tt(ZH.v(c), P.banks[bks[c]].v(0, 126, 128), V(chs[c].ap[:, 126:128], chs[c].bufs), ALU.mult)
                        P.release(bks[c])
                        P.ts(ZH.v(c), ZH.v(c), FLG.v(0), ALU.mult)
            P.reset(mk)

        def l1_mix_B(KTA, VVA, KTB, VVB):
            mk = P.mark()
            s32 = RR([P.alloc(1, TT, F32) for _ in range(3)])
            s16 = RR([P.alloc(1, TT, BF16) for _ in range(5)])
            Ht = P.alloc(8, TT, BF16)
            MIX = P.alloc(8, TT, BF16)
            QT = P.alloc(4, TT, BF16)
            Z = P.alloc(4, 2 + TT, BF16)
            WI = [P.alloc(8, 512, BF16), P.alloc(8, 512, BF16)]
            wq = [0]

            def nextw():
                w_ = WI[wq[0] % 2]
                wq[0] += 1
                return w_
            COS = P.alloc(1, TT, F32)
            SIN = P.alloc(1, TT, F32)
            MT = [P.alloc(1, TT, BF16), P.alloc(1, TT, BF16)]
            QH = [P.alloc(1, TT, BF16), P.alloc(1, TT, BF16)]
            P.memset(MT[0].all(), 0.0)
            P.memset(MT[1].all(), 0.0)
            G = P.alloc(1, 128, F32)
            G2 = P.alloc(1, 128, F32)
            MB = P.alloc(1, 512, F32)
            M1 = P.alloc(1, 8, F32)
            RD = P.alloc(1, 4, F32)
            YD = V(Ht.full.bitcast(F32), Ht.allbufs())
            for t in range(NT):
                lo = t * TT
                load_cs(1, t, COS, SIN)
                norm_tile(lo, A1[1], lambda k: modc(1, 0, k), lambda k: Ht.v(k), s32, s16)
                hk = lambda k: Ht.v(k)
                for c in range(4):
                    P.copy(Z.v(c, 0, 2), ZH.v(c), eng="act")
                w = nextw()
                stream_cols(o_w_in, 0, 512, w)
                bks = proj_group(hk, w, 4)
                chs = []
                for c in range(4):
                    s = s16.get().v(0)
                    P.act(s, P.banks[bks[c]].v(0), AF.Identity)
                    P.release(bks[c])
                    chs.append(s)
                w = nextw()
                stream_cols(o_w_in, 1024, 1536, w)
                bks = proj_group(hk, w, 4)
                for c in range(4):
                    P.tt(Z.v(c, 2, 2 + TT), P.banks[bks[c]].v(0), chs[c], ALU.mult)
                    P.release(bks[c])
                    P.copy(ZH.v(c), Z.v(c, TT, TT + 2), eng="act")
                w = nextw()
                stream_cols(o_w_in, 512, 1024, w)
                bks = proj_group(hk, w, 4)
                for c in range(4):
                    cv = s32.get().v(0)
                    for j in range(3):
                        wcol = po_col("ccw", c * 3 + j)
                        if j == 0:
                            P.ts(cv, Z.v(c, 0, TT), wcol, ALU.mult)
                        else:
                            P.stt(cv, Z.v(c, j, j + TT), wcol, cv, ALU.mult, ALU.add)
                    P.tt(MIX.v(c), P.banks[bks[c]].v(0), cv, ALU.mult)
                    P.release(bks[c])
                w = nextw()
                stream_cols(o_w_in, 1536, 2048, w)
                bks = proj_group(hk, w, 4)
                qk_finish(bks, lambda c: QT.v(c), po_col("qn"), None, s32, s16, COS.v(0), SIN.v(0))
                kv_tile(hk, t, KTB, VVB, WI, wq, s32, s16, COS.v(0), SIN.v(0), part="k")
                kmean_tile(KTB, 1, t)
                bk = P.acquire()
                gb = P.banks[bk]
                for s in range(4):
                    for h in range(8):
                        c, pb = h // 2, (h % 2) * 64
                        col = s * 128 + h * 16
                        P.mm(gb.v(0, col, col + 16), QT.v(c, s * 128, (s + 1) * 128), KMH[h % 2].v(c, 0, 16))
                kv_tile(hk, t, KTB, VVB, WI, wq, s32, s16, COS.v(0), SIN.v(0), part="v")
                for s in range(4):
                    own = 8 + 2 * t + s // 2
                    def v3(Tn, j0, j1):
                        so = 0 if Tn.N == 128 else s * 128
                        ap = Tn.full[:, so:so + 128].rearrange("p (h j) -> p h j", h=8)[:, :, j0:j1]
                        return V(ap, Tn.allbufs())
                    def bb(j0, j1):
                        return V(BLKB.full.unsqueeze(1).to_broadcast([128, 8, 16])[:, :, j0:j1], BLKB.allbufs())
                    def m1b():
                        return V(M1.full.unsqueeze(2).to_broadcast([128, 8, own]), M1.allbufs())
                    P.memset(v3(MB, 0, 16), -BIG)
                    P.memset(v3(MB, own, own + 1), 0.0)
                    P.tt(v3(G, 0, own), v3(gb, 0, own), bb(0, own), ALU.add)
                    P.copy(v3(G2, 0, own), v3(G, 0, own))
                    for rep in range(3):
                        P.reduce(M1.all(), v3(G2, 0, own), ALU.max)
                        if rep < 2:
                            e = v3(MB, 0, own)
                            P.tt(e, v3(G2, 0, own), m1b(), ALU.is_equal)
                            P.stt(v3(G2, 0, own), e, -BIG, v3(G2, 0, own), ALU.mult, ALU.add)
                    e = v3(MB, 0, own)
                    P.tt(e, v3(G, 0, own), m1b(), ALU.is_ge)
                    P.ts(e, e, -1.0, ALU.add, BIG, ALU.mult)
                    P.tt(e, e, bb(0, own), ALU.add)
                P.release(bk)
                nkc = 16 + 4 * (t + 1)
                for h in range(8):
                    c, pb = h // 2, (h % 2) * 64
                    mt = MT[h % 2]
                    bk = P.acquire()
                    for s in range(4):
                        col = s * 128 + h * 16
                        P.transpose(P.banks[bk].v(0, s * 128, (s + 1) * 128, 0, 16), MB.v(0, col, col + 16), ident32)
                    P.act(mt.v(0, 0, TT, 0, 16), P.banks[bk].v(0, 0, TT, 0, 16), AF.Identity)
                    P.release(bk)
                    qh = QH[h % 2]
                    P.ts(qh.all(), QT.v(c), hmask[h % 2], ALU.mult)
                    abk = [P.acquire() for _ in range(4)]

                    def s_group(kc):
                        if kc < 16:
                            kt, kl = KTA, kc
                        else:
                            kt, kl = KTB, kc - 16
                        bs = P.acquire()
                        S = P.banks[bs].v(0)
                        diag = kc >= 16 + 4 * t
                        P.mm(S, kt.v(c, kl * 128, (kl + 1) * 128), qh.all(), start=True, stop=False)
                        P.mm(S, EJ.v(kc // 2), mt.all(), start=False, stop=not diag)
                        if diag:
                            ci = kc - 16 - 4 * t
                            P.mm(S, identb, CC.v(0, 384 - ci * 128, 384 - ci * 128 + TT), start=False, stop=True)
                        return bs

                    LOOK = 2
                    pend = {}
                    for kc in range(min(LOOK, nkc)):
                        pend[kc] = s_group(kc)
                    for kc in range(nkc):
                        bs = pend.pop(kc)
                        pt = s16.get().v(0)
                        P.act(pt, P.banks[bs].v(0), AF.Exp, scale=0.125)
                        P.release(bs)
                        if kc + LOOK < nkc:
                            pend[kc + LOOK] = s_group(kc + LOOK)
                        vv, kl = (VVA, kc) if kc < 16 else (VVB, kc - 16)
                        for sq_ in range(4):
                            P.mm(P.banks[abk[sq_]].v(0, 0, 65), V(pt.ap[:, sq_ * 128:(sq_ + 1) * 128], pt.bufs),
                                 vv.v(kl, h * 65, h * 65 + 65), start=(kc == 0), stop=(kc == nkc - 1))
                    for sq_ in range(4):
                        P.recip(RD.v(0, sq_, sq_ + 1), P.banks[abk[sq_]].v(0, 64, 65))
                        P.ts(V(YD.ap[:, sq_ * 512 + h * 64: sq_ * 512 + h * 64 + 64], YD.bufs),
                             P.banks[abk[sq_]].v(0, 0, 64), RD.v(0, sq_, sq_ + 1), ALU.mult)
                        P.release(abk[sq_])
                for c in range(4):
                    bk = P.acquire()
                    for sq_ in range(4):
                        P.transpose(P.banks[bk].v(0, sq_ * 128, (sq_ + 1) * 128),
                                    V(YD.ap[:, sq_ * 512 + c * 128: sq_ * 512 + (c + 1) * 128], YD.bufs), ident32)
                    P.act(MIX.v(4 + c), P.banks[bk].v(0), AF.Identity)
                    P.release(bk)
                out_proj(o_w_out, MIX, lambda d: modc(1, 2, d), lo, nextw)
            P.reset(mk)

        def moe_B():
            mk = P.mark()
            H = P.alloc(8, T, BF16, tile=TT)
            WBs = [P.alloc(1, T, F32, tile=TT), P.alloc(1, T, F32, tile=TT)]
            D16 = [P.alloc(1, 128, F32) for _ in range(16)]
            mk2 = P.mark()
            s32 = RR([P.alloc(1, TT, F32) for _ in range(3)])
            s16 = RR([P.alloc(1, TT, BF16) for _ in range(3)])
            L8 = P.alloc(1, 32, F32)
            L8b = P.alloc(1, 32, F32)
            SEL = P.alloc(1, 32, F32)
            M4 = P.alloc(1, 4, F32)
            for t in range(NT):
                lbk = []

                def h32cb(k, tv):
                    if k == 0:
                        for s in range(4):
                            lbk.append(P.acquire())
                    h32 = s32.get().v(0)
                    P.act(h32, tv, AF.Identity, scale=A2[1](k), bias=modc(1, 3, k))
                    for s in range(4):
                        P.mm(P.banks[lbk[s]].v(0, 0, 8), V(h32.ap[:, s * 128:(s + 1) * 128], h32.bufs), RW.v(k),
                             start=(k == 0), stop=(k == 7))
                norm_tile(t * TT, A2[1], lambda k: modc(1, 3, k), lambda k, t=t: H.v(k, t * TT, (t + 1) * TT), s32, s16, h32cb)

                def v3(Tn, n=8):
                    return V(Tn.full[:, 0:4 * n].rearrange("p (s e) -> p s e", s=4), Tn.allbufs())
                rb3 = V(POt.full[:, PO_OFF["rb"]:PO_OFF["rb"] + 8].unsqueeze(1).to_broadcast([128, 4, 8]), POt.allbufs())
                m4b = lambda: V(M4.full.unsqueeze(2).to_broadcast([128, 4, 8]), M4.allbufs())
                for s in range(4):
                    P.tt(L8.v(0, s * 8, s * 8 + 8), P.banks[lbk[s]].v(0, 0, 8), po_col("rb", 0, 8), ALU.add)
                    P.release(lbk[s])
                P.reduce(M4.all(), v3(L8), ALU.max)
                P.tt(v3(SEL), v3(L8), m4b(), ALU.is_equal)
                P.stt(v3(L8b), v3(SEL), -BIG, v3(L8), ALU.mult, ALU.add)
                P.tt(v3(L8), v3(L8), m4b(), ALU.subtract)
                P.act(L8.all(), L8.all(), AF.Exp)
                P.reduce(M4.all(), v3(L8b), ALU.max)
                P.tt(v3(L8b), v3(L8b), m4b(), ALU.is_ge)
                P.tt(v3(SEL), v3(SEL), v3(L8b), ALU.add)
                P.tt(v3(L8), v3(L8), v3(SEL), ALU.mult)
                P.reduce(M4.all(), v3(L8), ALU.add)
                P.recip(M4.all(), M4.all())
                P.tt(V(WTOK.full[:, t * 32:(t + 1) * 32].rearrange("p (s e) -> p s e", s=4), WTOK.allbufs()),
                     v3(L8), m4b(), ALU.mult)
            P.reset(mk2)
            ws = ffn_bufs(True)

            def wb_diag(e):
                for st in range(16):
                    P.ts(D16[st].v(0), ident32, WTOK.v(0, st * 8 + e, st * 8 + e + 1), ALU.mult)

            def wb_mm(e):
                for t in range(NT):
                    bk = P.acquire()
                    for s in range(4):
                        P.mm(P.banks[bk].v(0, s * 128, (s + 1) * 128), ones32, D16[t * 4 + s].v(0))
                    P.act(WBs[e % 2].v(0, t * TT, (t + 1) * TT), P.banks[bk].v(0), AF.Identity)
                    P.release(bk)

            for e in range(8):
                wb_diag(e)
                wb_mm(e)
                ffn_like(H, m_wg[e * 1024:(e + 1) * 1024, :], m_wu[e * 1024:(e + 1) * 1024, :],
                         m_wd[e * 3584:(e + 1) * 3584, :], 28, lambda d: modc(1, 5, d), WBs[e % 2], ws)
            ffn_flush(ws)
            P.reset(mk)

        xr = lambda xd: xd.rearrange("(k p) t -> p k t", p=128)
        for t_ in range(NT):
            P.dma(X.vc(0, 8, t_ * TT, (t_ + 1) * TT), xr(xA)[:, :, t_ * TT:(t_ + 1) * TT])
        KVTOP = ARENA_BYTES
        if stage >= 1:
            l0_mix(0)
        compute_mod(1)
        if stage >= 2:
            l0_ffn()
        KTA = VVA = KTB = VVB = None
        if stage >= 3:
            KTA = P.alloc(4, T, BF16, tile=256, at=KVTOP - 16384)
            VVA = P.alloc(16, 520, BF16, at=KVTOP - 16384 - 16640)
            P.memset(V(VVA.full.rearrange("p (g e) -> p g e", e=65)[:, :, 64:65], VVA.allbufs()), 1.0)
            l1_kv_A(KTA, VVA)
        for t_ in range(NT):
            P.dma(X.vc(0, 8, t_ * TT, (t_ + 1) * TT), xr(xB)[:, :, t_ * TT:(t_ + 1) * TT])
        if stage >= 1:
            l0_mix(1)
        if stage >= 2:
            l0_ffn()
        if stage >= 4:
            KTB = P.alloc(4, T, BF16, tile=256, at=KVTOP - 33024 - 16384)
            VVB = P.alloc(16, 520, BF16, at=KVTOP - 33024 - 16384 - 16640)
            P.memset(V(VVB.full.rearrange("p (g e) -> p g e", e=65)[:, :, 64:65], VVB.allbufs()), 1.0)
            l1_mix_B(KTA, VVA, KTB, VVB)
            for tns in (KTA, VVA, KTB, VVB):
                P.free(tns)
        if stage >= 5:
            moe_B()
        outs = []
        for t_ in range(NT):
            outs.append(P.dma(xr(out_d)[:, :, t_ * TT:(t_ + 1) * TT], X.vc(0, 8, t_ * TT, (t_ + 1) * TT)))
        P.finish(outs)
        P.emit()
    return nc
from concourse.bass_utils import run_bass_kernel_spmd

ROPE_THETA = 10000.0
STAGE = 99
_NC_CACHE = {}


def _cols(v, n):
    return np.ascontiguousarray(np.asarray(v, np.float32).reshape(n, 128).T)


def _const_arrays():
    ident = np.eye(128, dtype=np.float32)
    ones = np.ones((128, 128), np.float32)
    hm = np.zeros((128, 2), np.float32)
    hm[:64, 0] = 1.0
    hm[64:, 1] = 1.0
    c32 = np.concatenate([ident, ones, hm], axis=1)
    blk = np.zeros((128, 128), np.float32)
    blk[:64, :64] = 1.0 / 64
    blk[64:, 64:] = 1.0 / 64
    rperm = np.zeros((128, 128), np.float32)
    for m in range(128):
        p = 64 * (m // 64) + ((m % 64) + 32) % 64
        rperm[p, m] = 1.0
    cb = np.concatenate([ident, ones / 1024.0, ones / 512.0, blk, ones, rperm], axis=1)
    kk = np.arange(128)[:, None]
    uu = np.arange(896)[None, :] - 384
    cc = np.where(kk <= uu, 0.0, -BIG).astype(np.float32)
    ej = np.zeros((128, 16, 128), np.float32)
    for j in range(16):
        ej[j, j, :] = 1.0
    return c32, cb, cc, ej.reshape(128, 2048)


def _rope_tables(sh):
    half = 32
    inv = (np.float32(ROPE_THETA) ** (-np.arange(half, dtype=np.float32) / np.float32(half))).astype(np.float32)
    slots = np.arange(2 * T)
    if sh == 1:
        pos = slots
    else:
        pos = np.where(slots >= T, slots - T, 0)
    ang = pos.astype(np.float32)[None, :] * inv[:, None]
    cos = np.cos(ang).astype(np.float32)
    sin = np.sin(ang).astype(np.float32)
    i = np.arange(128) % 64
    cosT = cos[i % 32]
    sinT = np.where((i < 32)[:, None], -sin[i % 32], sin[i % 32])
    return np.ascontiguousarray(cosT, np.float32), np.ascontiguousarray(sinT, np.float32)


def _prep(inp):
    g = lambda n: np.asarray(inp[n], np.float32)
    x = g("x")
    c = g("c")
    caw = g("e_conv_a_w")[0]
    cbw = g("e_conv_b_w")[0]
    ccw = g("o_conv_c_w")[0]
    pe = np.concatenate([
        _cols(g("e_norm_mix")[0], 8), _cols(g("e_norm_ffn")[0], 8), _cols(g("e_ada_b")[0], 48),
        np.ascontiguousarray(caw.T.reshape(4, 128, 31).transpose(1, 0, 2)).reshape(128, 124),
        _cols(g("e_conv_a_b")[0], 4), _cols(g("e_ln_a_g")[0], 4), _cols(g("e_ln_a_b")[0], 4),
        np.ascontiguousarray(cbw.T.reshape(4, 128, 4).transpose(1, 0, 2)).reshape(128, 16),
        _cols(g("e_conv_b_b")[0], 4), _cols(g("e_lru_ba")[0], 4), _cols(g("e_lru_bx")[0], 4),
        _cols(g("e_lru_lambda")[0], 4)], axis=1)
    assert pe.shape[1] == NPE
    po = np.concatenate([
        _cols(g("o_norm_mix")[0], 8), _cols(g("o_norm_ffn")[0], 8), _cols(g("o_ada_b")[0], 48),
        np.ascontiguousarray(ccw.T.reshape(4, 128, 3).transpose(1, 0, 2)).reshape(128, 12),
        np.tile(g("o_q_norm")[0], 2)[:, None], np.tile(g("o_k_norm")[0], 2)[:, None],
        np.broadcast_to(g("o_router_b")[0][None, :], (128, 8))], axis=1)
    assert po.shape[1] == NPO
    lru = np.zeros((128, 8, 128), np.float32)
    wa = g("e_lru_wa")[0]
    wx = g("e_lru_wx")[0]
    for cch in range(4):
        for hh in range(2):
            lru[hh * 64:(hh + 1) * 64, cch, hh * 64:(hh + 1) * 64] = wa[2 * cch + hh]
            lru[hh * 64:(hh + 1) * 64, 4 + cch, hh * 64:(hh + 1) * 64] = wx[2 * cch + hh]
    c32, cb, cc, ej = _const_arrays()
    shared = {
        "pe": np.ascontiguousarray(pe), "po": np.ascontiguousarray(po),
        "c32": c32, "cb": cb, "cc": cc, "ej": ej,
        "e_ada_w": g("e_ada_w")[0], "o_ada_w": g("o_ada_w")[0],
        "e_w_in": g("e_w_in")[0], "e_w_out": g("e_w_out")[0], "lru_w": lru.reshape(128, 1024),
        "e_ffn_wg": g("e_ffn_wg")[0], "e_ffn_wu": g("e_ffn_wu")[0], "e_ffn_wd": g("e_ffn_wd")[0],
        "o_w_in": g("o_w_in")[0], "o_w_out": g("o_w_out")[0], "o_router_w": g("o_router_w")[0],
    }
    if STAGE >= 5:
        shared["o_moe_wg"] = g("o_moe_wg")[0].reshape(8 * 1024, 3584)
        shared["o_moe_wu"] = g("o_moe_wu")[0].reshape(8 * 1024, 3584)
        shared["o_moe_wd"] = g("o_moe_wd")[0].reshape(8 * 3584, 1024)
    tabs = [_rope_tables(0), _rope_tables(1)]
    maps = []
    for core in range(8):
        b, sh = core // 2, core % 2
        m = dict(shared)
        if sh == 1:
            m["xA"] = np.ascontiguousarray(x[b, :T].T)
        else:
            m["xA"] = np.zeros((1024, T), np.float32)
        m["xB"] = np.ascontiguousarray(x[b, sh * T:(sh + 1) * T].T)
        m["cvec"] = _cols(c[b], 8)
        m["flag"] = np.full((128, 1), float(sh), np.float32)
        bb = np.zeros((128, 16), np.float32)
        if sh == 0:
            bb[:, :8] = -BIG
        m["blkb"] = bb
        m["cos"], m["sin"] = tabs[sh]
        maps.append(m)
    return maps


def kernel(**inputs):
    maps = _prep(inputs)
    if STAGE not in _NC_CACHE:
        _NC_CACHE[STAGE] = build_program(STAGE)
    nc = _NC_CACHE[STAGE]
    res = run_bass_kernel_spmd(nc, maps, core_ids=list(range(8)))
    out = np.empty((4, 2 * T, 1024), np.float32)
    for core in range(8):
        b, sh = core // 2, core % 2
        out[b, sh * T:(sh + 1) * T, :] = np.asarray(res.results[core]["out"], np.float32).T
    return out
```
